# Optimizing a Trainium2 kernel written in Bass

```python
import math
import jax
import jax.numpy as jnp
from jax import lax
import numpy as np

D_MODEL = 2048
BATCH = 2
SEQ = 4096
DEPTH = 2

GRID_W = 64
CTX_LEN = 256
F32 = jnp.float32

HY_WIDTH = D_MODEL // 4
LRU_WIDTH = D_MODEL // 4
ATT_WIDTH = D_MODEL // 2
D_MIX = HY_WIDTH + LRU_WIDTH + ATT_WIDTH

HY_GROUPS = 4
HY_ORDER = 2
HY_CONV = 3
HY_BANDS = 8
HY_EMB = 1 + 2 * HY_BANDS
HY_FILTER_HIDDEN = 64
HY_TARGET = 1e-2
HY_SHORT_DECAY_PCT = 0.3
HY_LONG_DECAY_PCT = 1.5
HY_MAX_DECAY = math.log(HY_TARGET) / HY_SHORT_DECAY_PCT
HY_MIN_DECAY = math.log(HY_TARGET) / HY_LONG_DECAY_PCT
HY_IN = (HY_ORDER + 1) * HY_WIDTH

LRU_HEADS = 8
LRU_HEAD_DIM = LRU_WIDTH // LRU_HEADS
LRU_CONV = 4
LRU_C = 8.0
LRU_IN = 2 * LRU_WIDTH

ATT_HEADS = 4
ATT_V_DIM = ATT_WIDTH // ATT_HEADS
ATT_HEAD_DIM = ATT_V_DIM // 2
ATT_QK = ATT_HEADS * 2 * ATT_HEAD_DIM
ATT_IN = 2 * ATT_QK + ATT_WIDTH
Q_BLOCK = 128
ROPE_THETA = 10000.0
ROPE_AXIS_DIM = ATT_HEAD_DIM // 2

D_IN = HY_IN + LRU_IN + ATT_IN

N_EXPERTS = 64
TOP_K = 8
D_EXPERT = 512
D_SHARED = 512
ROUTED_SCALE = 2.5
EXPERT_BLOCK = 128

LN_EPS = 1e-5
DEEPNORM_ALPHA = (2 * DEPTH) ** 0.25
DEEPNORM_BETA = (8 * DEPTH) ** -0.25

kernel_name = 'hybrid_hyena_rglru_diffattn_moe_dit'


def layer_norm(x, gain=None, bias=None):
    xf = x.astype(F32)
    mu = jnp.mean(xf, axis=-1, keepdims=True)
    var = jnp.mean(jnp.square(xf - mu), axis=-1, keepdims=True)
    y = (xf - mu) * lax.rsqrt(var + LN_EPS)
    if gain is not None:
        y = y * gain + bias
    return y.astype(x.dtype)


def modulate(x, shift, scale):
    return layer_norm(x) * (1.0 + scale) + shift


def depthwise_conv(x, w, b, pad_left, pad_right):
    y = lax.conv_general_dilated(
        x, w[:, None, :].astype(x.dtype), window_strides=(1,),
        padding=[(pad_left, pad_right)], dimension_numbers=('NWC', 'WIO', 'NWC'),
        feature_group_count=x.shape[-1])
    return y + b


def split_groups(u):
    return u[..., :HY_IN], u[..., HY_IN:HY_IN + LRU_IN], u[..., HY_IN + LRU_IN:]


def hyena_filter_spectrum(n, w1, b1, w2, b2, w3, freq):
    pos = jnp.arange(n, dtype=F32)
    t = (pos / max(n - 1, 1))[:, None]
    bands = jnp.linspace(1e-4, HY_BANDS - 1, HY_BANDS, dtype=F32)
    ang = (2.0 * math.pi / n) * pos[:, None] * bands
    feats = jnp.concatenate([t, jnp.cos(ang), -jnp.sin(ang)], axis=-1)
    hid = jnp.sin(freq * (feats @ w1 + b1))
    hid = jnp.sin(freq * (hid @ w2 + b2))
    filt = (hid @ w3).astype(F32).reshape(n, 2, HY_ORDER, HY_WIDTH)
    decay = jnp.abs(jnp.linspace(HY_MIN_DECAY, HY_MAX_DECAY, HY_WIDTH, dtype=F32))
    filt = filt * jnp.exp(-t * decay)[:, None, None, :]
    fwd, bwd = filt[:, 0], filt[:, 1]
    two_sided = jnp.concatenate([fwd, jnp.zeros_like(fwd[:1]), jnp.flip(bwd[1:], axis=0)], axis=0)
    two_sided = two_sided / jnp.sum(jnp.abs(two_sided), axis=0, keepdims=True)
    return jnp.fft.rfft(two_sided, axis=0)


def hyena_mixer(u, conv_w, conv_b, w1, b1, w2, b2, w3, freq, skip):
    n = u.shape[1]
    u = depthwise_conv(u, conv_w, conv_b, HY_CONV // 2, HY_CONV - 1 - HY_CONV // 2).astype(F32)
    gates = (u[..., :HY_WIDTH], u[..., HY_WIDTH:2 * HY_WIDTH])
    z = u[..., 2 * HY_WIDTH:]
    spec = hyena_filter_spectrum(n, w1, b1, w2, b2, w3, freq)
    for o in range(HY_ORDER):
        zc = jnp.fft.irfft(jnp.fft.rfft(z, n=2 * n, axis=1) * spec[None, :, o], n=2 * n, axis=1)[:, :n]
        z = gates[o] * (zc + skip[o] * z)
    return z


def rglru_coeffs(xr, wa, ba, wx, bx, lam):
    bsz, n, _ = xr.shape
    xf = xr.astype(F32)
    xh = xf.reshape(bsz, n, LRU_HEADS, LRU_HEAD_DIM)
    r = jax.nn.sigmoid(jnp.einsum('blhi,hij->blhj', xh, wa).reshape(bsz, n, LRU_WIDTH) + ba)
    i = jax.nn.sigmoid(jnp.einsum('blhi,hij->blhj', xh, wx).reshape(bsz, n, LRU_WIDTH) + bx)
    log_a = -LRU_C * r * jax.nn.softplus(-lam)
    a = jnp.exp(log_a)
    b = jnp.sqrt(-jnp.expm1(2.0 * log_a)) * (i * xf)
    return a, b


def _affine_combine(left, right):
    a_l, b_l = left
    a_r, b_r = right
    return a_l * a_r, a_r * b_l + b_r


def linear_scan(a, b, h0, reverse):
    if reverse:
        a, b = jnp.flip(a, axis=1), jnp.flip(b, axis=1)
    a_cum, b_cum = lax.associative_scan(_affine_combine, (a, b), axis=1)
    h = a_cum * h0[:, None, :] + b_cum
    return jnp.flip(h, axis=1) if reverse else h


def rglru_mixer(u_lat, u_ctx, conv_w, conv_b, wa, ba, wx, bx, lam, need_ctx):
    def recurrent_input(u):
        return depthwise_conv(u[..., LRU_WIDTH:], conv_w, conv_b, LRU_CONV // 2, LRU_CONV - 1 - LRU_CONV // 2)
    xr_lat, xr_ctx = recurrent_input(u_lat), recurrent_input(u_ctx)
    h_lat = jnp.zeros(xr_lat.shape, F32)
    ctx_states = []
    for d, reverse in enumerate((False, True)):
        a_c, b_c = rglru_coeffs(xr_ctx, wa[d], ba[d], wx[d], bx[d], lam[d])
        hs_c = linear_scan(a_c, b_c, jnp.zeros_like(a_c[:, 0]), reverse)
        h_end = hs_c[:, 0] if reverse else hs_c[:, -1]
        a_l, b_l = rglru_coeffs(xr_lat, wa[d], ba[d], wx[d], bx[d], lam[d])
        h_lat = h_lat + linear_scan(a_l, b_l, h_end, reverse)
        ctx_states.append(hs_c)
    y_lat = h_lat * jax.nn.gelu(u_lat[..., :LRU_WIDTH].astype(F32))
    y_ctx = None
    if need_ctx:
        y_ctx = (ctx_states[0] + ctx_states[1]) * jax.nn.gelu(u_ctx[..., :LRU_WIDTH].astype(F32))
    return y_lat, y_ctx


def axial_rope_tables(n):
    rows = n // GRID_W
    row = jnp.broadcast_to(jnp.arange(rows, dtype=F32)[:, None], (rows, GRID_W)).reshape(-1)
    col = jnp.broadcast_to(jnp.arange(GRID_W, dtype=F32)[None, :], (rows, GRID_W)).reshape(-1)
    inv = ROPE_THETA ** (-jnp.arange(0, ROPE_AXIS_DIM, 2, dtype=F32) / ROPE_AXIS_DIM)
    ang = jnp.stack([row[:, None] * inv, col[:, None] * inv], axis=1)
    return jnp.cos(ang), jnp.sin(ang)


def apply_axial_rope(x, cos, sin):
    xs = x.reshape(x.shape[:-1] + (2, 2, ROPE_AXIS_DIM // 2))
    x1, x2 = xs[..., 0, :], xs[..., 1, :]
    cb, sb = cos[None, :, None, None], sin[None, :, None, None]
    out = jnp.stack([x1 * cb - x2 * sb, x2 * cb + x1 * sb], axis=-2)
    return out.reshape(x.shape)


def diff_attend(q, k, v, lam):
    s = jnp.einsum('bqhmd,bkhmd->bhmqk', q, k).astype(F32) * (ATT_HEAD_DIM ** -0.5)
    p = jax.nn.softmax(s, axis=-1)
    w = p[:, :, 0] - lam * p[:, :, 1]
    return jnp.einsum('bhqk,bkhe->bqhe', w, v.astype(F32))


def head_rms_norm(o, gain, lam_init):
    of = o.astype(F32)
    y = of * lax.rsqrt(jnp.mean(of * of, axis=-1, keepdims=True) + LN_EPS) * gain * (1.0 - lam_init)
    return y.reshape(o.shape[0], o.shape[1], -1)


def diff_attention_mixer(u_lat, u_ctx, lam_vecs, subln_gain, lam_init, need_ctx):
    bsz, n_lat, _ = u_lat.shape

    def split_qkv(u):
        bb, n, _ = u.shape
        q = u[..., :ATT_QK].reshape(bb, n, ATT_HEADS, 2, ATT_HEAD_DIM)
        k = u[..., ATT_QK:2 * ATT_QK].reshape(bb, n, ATT_HEADS, 2, ATT_HEAD_DIM)
        v = u[..., 2 * ATT_QK:].reshape(bb, n, ATT_HEADS, ATT_V_DIM)
        return q, k, v

    q_l, k_l, v_l = split_qkv(u_lat)
    q_c, k_c, v_c = split_qkv(u_ctx)
    cos, sin = axial_rope_tables(n_lat)
    q_l = apply_axial_rope(q_l, cos, sin)
    k_l = apply_axial_rope(k_l, cos, sin)
    lv = lam_vecs.astype(F32)
    lam = jnp.exp(jnp.sum(lv[0] * lv[1])) - jnp.exp(jnp.sum(lv[2] * lv[3])) + lam_init
    k_all = jnp.concatenate([k_c, k_l], axis=1)
    v_all = jnp.concatenate([v_c, v_l], axis=1)
    n_blk = n_lat // Q_BLOCK
    q_blocks = jnp.moveaxis(q_l.reshape(bsz, n_blk, Q_BLOCK, ATT_HEADS, 2, ATT_HEAD_DIM), 1, 0)
    o = lax.map(lambda qb: diff_attend(qb, k_all, v_all, lam), q_blocks)
    o_lat = jnp.moveaxis(o, 0, 1).reshape(bsz, n_lat, ATT_HEADS, ATT_V_DIM)
    y_lat = head_rms_norm(o_lat, subln_gain, lam_init)
    y_ctx = head_rms_norm(diff_attend(q_c, k_c, v_c, lam), subln_gain, lam_init) if need_ctx else None
    return y_lat, y_ctx


def moe_ffn(h, router_w, router_b, w_gate, w_up, w_down, sh_gate, sh_up, sh_down):
    n_tok, d = h.shape
    scores = jax.nn.sigmoid((h @ router_w).astype(F32))
    _, top_idx = lax.top_k(scores + router_b.astype(F32), TOP_K)
    top_s = jnp.take_along_axis(scores, top_idx, axis=1)
    top_w = top_s / jnp.sum(top_s, axis=-1, keepdims=True) * ROUTED_SCALE
    n_assign = n_tok * TOP_K
    flat_e = top_idx.reshape(-1)
    order = jnp.argsort(flat_e)
    sorted_e = flat_e[order]
    counts = jnp.bincount(flat_e, length=N_EXPERTS)
    padded = (counts + EXPERT_BLOCK - 1) // EXPERT_BLOCK * EXPERT_BLOCK
    padded_end = jnp.cumsum(padded)
    start = jnp.cumsum(counts) - counts
    dest = (padded_end - padded)[sorted_e] + jnp.arange(n_assign) - start[sorted_e]
    n_blocks = -(-n_assign // EXPERT_BLOCK) + N_EXPERTS
    n_rows = n_blocks * EXPERT_BLOCK
    row_tok = jnp.zeros((n_rows,), jnp.int32).at[dest].set((order // TOP_K).astype(jnp.int32))
    row_w = jnp.zeros((n_rows,), F32).at[dest].set(top_w.reshape(-1)[order])
    block_e = jnp.minimum(
        jnp.searchsorted(padded_end, jnp.arange(n_blocks) * EXPERT_BLOCK, side='right'), N_EXPERTS - 1)

    def expert_block(args):
        tok, wgt, e = args
        xb = h[tok]
        act = jax.nn.silu(xb @ w_gate[e]) * (xb @ w_up[e])
        return (act @ w_down[e]).astype(F32) * wgt[:, None]

    y = lax.map(expert_block, (row_tok.reshape(n_blocks, EXPERT_BLOCK),
                               row_w.reshape(n_blocks, EXPERT_BLOCK), block_e))
    routed = jnp.zeros((n_tok, d), F32).at[row_tok].add(y.reshape(n_rows, d))
    shared = (jax.nn.silu(h @ sh_gate) * (h @ sh_up)) @ sh_down
    return routed + shared


def setup_inputs(seed: int = 0) -> dict:
    key = jax.random.key(seed)
    ks = iter(jax.random.split(key, 48))

    def nrm(shape, scale):
        return jax.random.normal(next(ks), shape, F32) * scale

    L, D = DEPTH, D_MODEL
    a_base = jax.random.uniform(next(ks), (L, 2, LRU_WIDTH), F32, 0.9, 0.999) ** (1.0 / LRU_C)
    return {
        'x': nrm((BATCH, SEQ, D), 1.0),
        'c': nrm((BATCH, D), 1.0),
        'ctx': nrm((BATCH, CTX_LEN, D), 1.0),
        'c_ctx': nrm((D,), 1.0),
        'ada_w': nrm((L, D, 6 * D), 0.5 * D ** -0.5),
        'ada_b': nrm((L, 6 * D), 0.02),
        'w_in': nrm((L, D, D_IN), D ** -0.5),
        'hy_conv_w': nrm((L, HY_CONV, HY_IN), HY_CONV ** -0.5),
        'hy_conv_b': nrm((L, HY_IN), 0.02),
        'hy_w1': nrm((L, HY_EMB, HY_FILTER_HIDDEN), HY_EMB ** -0.5),
        'hy_b1': nrm((L, HY_FILTER_HIDDEN), 0.1),
        'hy_w2': nrm((L, HY_FILTER_HIDDEN, HY_FILTER_HIDDEN), HY_FILTER_HIDDEN ** -0.5),
        'hy_b2': nrm((L, HY_FILTER_HIDDEN), 0.1),
        'hy_w3': nrm((L, HY_FILTER_HIDDEN, 2 * HY_ORDER * HY_WIDTH), HY_FILTER_HIDDEN ** -0.5),
        'hy_freq': 1.0 + nrm((L, HY_FILTER_HIDDEN), 0.1),
        'hy_skip': nrm((L, HY_ORDER, HY_WIDTH), 0.5),
        'lru_conv_w': nrm((L, LRU_CONV, LRU_WIDTH), LRU_CONV ** -0.5),
        'lru_conv_b': nrm((L, LRU_WIDTH), 0.02),
        'lru_wa': nrm((L, 2, LRU_HEADS, LRU_HEAD_DIM, LRU_HEAD_DIM), LRU_HEAD_DIM ** -0.5),
        'lru_ba': nrm((L, 2, LRU_WIDTH), 0.02),
        'lru_wx': nrm((L, 2, LRU_HEADS, LRU_HEAD_DIM, LRU_HEAD_DIM), LRU_HEAD_DIM ** -0.5),
        'lru_bx': nrm((L, 2, LRU_WIDTH), 0.02),
        'lru_lambda': jnp.log(a_base) - jnp.log1p(-a_base),
        'att_lambda': nrm((L, 4, ATT_HEAD_DIM), 0.1),
        'att_subln': 1.0 + nrm((L, ATT_V_DIM), 0.02),
        'w_out': nrm((L, D_MIX, D), DEEPNORM_BETA * D_MIX ** -0.5),
        'ln_g': 1.0 + nrm((L, 2, D), 0.02),
        'ln_b': nrm((L, 2, D), 0.02),
        'router_w': nrm((L, D, N_EXPERTS), D ** -0.5),
        'router_b': nrm((L, N_EXPERTS), 0.01),
        'exp_w_gate': nrm((L, N_EXPERTS, D, D_EXPERT), D ** -0.5),
        'exp_w_up': nrm((L, N_EXPERTS, D, D_EXPERT), D ** -0.5),
        'exp_w_down': nrm((L, N_EXPERTS, D_EXPERT, D), DEEPNORM_BETA * D_EXPERT ** -0.5),
        'sh_w_gate': nrm((L, D, D_SHARED), D ** -0.5),
        'sh_w_up': nrm((L, D, D_SHARED), D ** -0.5),
        'sh_w_down': nrm((L, D_SHARED, D), DEEPNORM_BETA * D_SHARED ** -0.5),
    }


def reference(x, c, ctx, c_ctx, ada_w, ada_b, w_in, hy_conv_w, hy_conv_b, hy_w1, hy_b1, hy_w2,
              hy_b2, hy_w3, hy_freq, hy_skip, lru_conv_w, lru_conv_b, lru_wa, lru_ba, lru_wx,
              lru_bx, lru_lambda, att_lambda, att_subln, w_out, ln_g, ln_b, router_w, router_b,
              exp_w_gate, exp_w_up, exp_w_down, sh_w_gate, sh_w_up, sh_w_down):
    x_lat, x_ctx = x, ctx
    for l in range(DEPTH):
        need_ctx = l < DEPTH - 1
        lam_init = 0.8 - 0.6 * math.exp(-0.3 * l)
        mod_lat = (jax.nn.silu(c) @ ada_w[l] + ada_b[l])[:, None, :]
        mod_ctx = (jax.nn.silu(c_ctx) @ ada_w[l] + ada_b[l])[None, None, :]
        sh1_l, sc1_l, g1_l, sh2_l, sc2_l, g2_l = jnp.split(mod_lat, 6, axis=-1)
        sh1_c, sc1_c, g1_c, sh2_c, sc2_c, g2_c = jnp.split(mod_ctx, 6, axis=-1)
        hy_args = (hy_conv_w[l], hy_conv_b[l], hy_w1[l], hy_b1[l], hy_w2[l], hy_b2[l],
                   hy_w3[l], hy_freq[l], hy_skip[l])

        hy_l, lru_l, att_l = split_groups(modulate(x_lat, sh1_l, sc1_l) @ w_in[l])
        hy_c, lru_c, att_c = split_groups(modulate(x_ctx, sh1_c, sc1_c) @ w_in[l])
        y_hy_l = hyena_mixer(hy_l, *hy_args)
        y_lru_l, y_lru_c = rglru_mixer(lru_l, lru_c, lru_conv_w[l], lru_conv_b[l], lru_wa[l], lru_ba[l],
                                       lru_wx[l], lru_bx[l], lru_lambda[l], need_ctx)
        y_att_l, y_att_c = diff_attention_mixer(att_l, att_c, att_lambda[l], att_subln[l], lam_init, need_ctx)
        mix_l = jnp.concatenate([y_hy_l, y_lru_l, y_att_l], axis=-1) @ w_out[l]
        x_lat = layer_norm(DEEPNORM_ALPHA * x_lat + g1_l * mix_l, ln_g[l, 0], ln_b[l, 0])
        if need_ctx:
            mix_c = jnp.concatenate([hyena_mixer(hy_c, *hy_args), y_lru_c, y_att_c], axis=-1) @ w_out[l]
            x_ctx = layer_norm(DEEPNORM_ALPHA * x_ctx + g1_c * mix_c, ln_g[l, 0], ln_b[l, 0])

        f_lat = modulate(x_lat, sh2_l, sc2_l).reshape(-1, D_MODEL)
        n_lat_tok = f_lat.shape[0]
        if need_ctx:
            tokens = jnp.concatenate([f_lat, modulate(x_ctx, sh2_c, sc2_c).reshape(-1, D_MODEL)], axis=0)
        else:
            tokens = f_lat
        ffn = moe_ffn(tokens, router_w[l], router_b[l], exp_w_gate[l], exp_w_up[l], exp_w_down[l],
                      sh_w_gate[l], sh_w_up[l], sh_w_down[l])
        x_lat = layer_norm(DEEPNORM_ALPHA * x_lat + g2_l * ffn[:n_lat_tok].reshape(x_lat.shape),
                           ln_g[l, 1], ln_b[l, 1])
        if need_ctx:
            x_ctx = layer_norm(DEEPNORM_ALPHA * x_ctx + g2_c * ffn[n_lat_tok:].reshape(x_ctx.shape),
                               ln_g[l, 1], ln_b[l, 1])
    return x_lat
```

```python
import math
import contextlib
import numpy as np
import ml_dtypes
import concourse.bass as bass
import concourse.mybir as mybir
from concourse.bass_utils import run_bass_kernel_spmd

F32 = mybir.dt.float32
BF16 = mybir.dt.bfloat16
I32 = mybir.dt.int32
AF = mybir.ActivationFunctionType
ALU = mybir.AluOpType
AX = mybir.AxisListType

D = 2048
NCTX = 256
NLAT = 4096
NTOK = NCTX + NLAT
NTT = NTOK // 128
DEPTH = 2
D_IN = 5632
LN_EPS = 1e-5
ALPHA = (2 * DEPTH) ** 0.25
NEXP = 64
DEXP = 512

SAME_SYNC = True
ARENA_WORDS = 45 * 1024


class Tok:
    __slots__ = ("w", "r")

    def __init__(self):
        self.w = None
        self.r = {}


class T(Tok):
    __slots__ = ("ap",)

    def __init__(self, ap):
        Tok.__init__(self)
        self.ap = ap

    def __getitem__(self, k):
        return self.ap[k]


class KB:
    ENGS = ("sp", "act", "dve", "pool", "pe")
    ATTR = {"sp": "sync", "act": "scalar", "dve": "vector", "pool": "gpsimd", "pe": "tensor"}
    NDMA = 6

    def __init__(self, io_in=(), io_out=()):
        self.nc = bass.Bass("TRN2", target_bir_lowering=False)
        self.st = contextlib.ExitStack()
        self.q = {e: [] for e in self.ENGS}
        self.waited = {e: {} for e in self.ENGS}
        self.cnt = {}
        self.sem = {}
        self.rr = {e: 0 for e in self.ENGS}
        self.cur = {}
        self.gen = {}
        for e in self.ENGS:
            self._mksem("s_" + e)
            self.cur["s_" + e] = "s_" + e
            self.gen["s_" + e] = 0
            if e in ("sp", "pool"):
                for i in range(self.NDMA):
                    self._mksem(f"d_{e}_{i}")
                    self.cur[f"d_{e}_{i}"] = f"d_{e}_{i}"
                    self.gen[f"d_{e}_{i}"] = 0
        self.arena = self.st.enter_context(self.nc.sbuf_tensor("arena", [128, ARENA_WORDS], F32))
        self.top = 0
        self.psum = self.st.enter_context(self.nc.psum_tensor("psum", [128, 8, 512], F32))
        self.bank = [T(self.psum[:, i, :]) for i in range(8)]
        self.io_in = set(io_in)
        self.io_out = set(io_out)
        self.inputs = {}
        self.outputs = {}
        self.drams = {}
        self.dtok = {}
        self.eps = self.tile((1,))
        self.memset(self.eps[:], LN_EPS, W=[self.eps])

    def _mksem(self, name):
        self.sem[name] = self.st.enter_context(self.nc.semaphore(name))
        self.cnt[name] = 0

    def tile(self, free, dtype=F32, parts=128):
        n = int(np.prod(free))
        words = n if dtype in (F32, I32) else (n + 1) // 2
        words = (words + 15) // 16 * 16
        off = self.top
        self.top += words
        assert self.top <= ARENA_WORDS, f"SBUF arena overflow {self.top}"
        ap = self.arena[:, off:off + words]
        if dtype != F32:
            ap = ap.bitcast(dtype)
        ap = ap[:, 0:n]
        if len(free) == 2:
            ap = ap.rearrange("p (a b) -> p a b", b=free[1])
        elif len(free) == 3:
            ap = ap.rearrange("p (a b c) -> p a b c", b=free[1], c=free[2])
        return T(ap)

    @contextlib.contextmanager
    def scope(self):
        mark = self.top
        yield
        self.barrier()
        self.top = mark

    def dram(self, name, shape, dtype):
        if name in self.drams:
            return self.drams[name]
        if name in self.io_in:
            t = self.nc.dram_tensor(name, list(shape), dtype, kind="ExternalInput")
            self.inputs[name] = (tuple(shape), dtype)
        elif name in self.io_out:
            t = self.nc.dram_tensor(name, list(shape), dtype, kind="ExternalOutput")
            self.outputs[name] = (tuple(shape), dtype)
        else:
            t = self.nc.dram_tensor(name, list(shape), dtype)
        self.drams[name] = t
        return t

    def inp(self, name, shape, dtype=F32):
        if name not in self.drams:
            self.io_in.add(name)
        return self.dram(name, shape, dtype)

    def tok(self, *key):
        t = self.dtok.get(key)
        if t is None:
            t = self.dtok[key] = Tok()
        return t

    def _wait(self, eng, evs):
        own = self.cur["s_" + eng]
        for sem, val in evs:
            if sem == own and (eng == "pe" or not SAME_SYNC):
                continue
            if self.waited[eng].get(sem, 0) >= val:
                continue
            self.waited[eng][sem] = val
            self.q[eng].append(("w", sem, val))

    def _deps(self, R, W):
        evs = []
        for t in R:
            if t.w is not None:
                evs.append(t.w)
        for t in W:
            if t.w is not None:
                evs.append(t.w)
            evs.extend(t.r.items())
        return evs

    def _mark(self, ev, R, W):
        sem, v = ev
        for t in R:
            if t.r.get(sem, 0) < v:
                t.r[sem] = v
        for t in W:
            t.w = ev
            t.r = {}

    def op(self, eng, fn, R=(), W=()):
        self._wait(eng, self._deps(R, W))
        sem = self.cur["s_" + eng]
        self.cnt[sem] += 1
        v = self.cnt[sem]
        self.q[eng].append(("o", fn, sem))
        self._mark((sem, v), R, W)

    def dma(self, eng, out, in_, R=(), W=(), **kw):
        i = self.rr[eng]
        self.rr[eng] = (i + 1) % self.NDMA
        sem = self.cur[f"d_{eng}_{i}"]
        evs = self._deps(R, W)
        if self.cnt[sem] > 0:
            evs.append((sem, self.cnt[sem]))
        self._wait(eng, evs)
        self.cnt[sem] += 16
        v = self.cnt[sem]
        self.q[eng].append(("d", out, in_, kw, sem))
        self._mark((sem, v), R, W)

    def barrier(self):
        evs = [(s, c) for s, c in self.cnt.items() if c > 0]
        for e in self.ENGS:
            self._wait(e, evs)
        for key in list(self.cur.keys()):
            name = self.cur[key]
            if self.cnt[name] > 12000:
                self.gen[key] += 1
                nn = f"{key}_g{self.gen[key]}"
                self._mksem(nn)
                self.cur[key] = nn

    def checkpoint(self):
        if max(self.cnt[self.cur[k]] for k in self.cur) > 12000:
            self.barrier()

    def emit(self):
        self.barrier()
        nc = self.nc
        with nc.Block() as block:
            for eng in self.ENGS:
                items = self.q[eng]
                sems = self.sem

                def body(e, items=items):
                    for it in items:
                        if it[0] == "w":
                            e.wait_ge(sems[it[1]], it[2])
                        elif it[0] == "o":
                            it[1](e).then_inc(sems[it[2]], 1)
                        else:
                            e.dma_start(out=it[1], in_=it[2], **it[3]).then_inc(sems[it[4]], 16)

                getattr(block, self.ATTR[eng])(body)
        return nc

    def mm(self, out, lhsT, rhs, start, stop, R, W):
        self.op("pe", lambda e: e.matmul(out, lhsT, rhs, start=start, stop=stop), R, W)

    def tr(self, out, in_, ident, R, W):
        self.op("pe", lambda e: e.transpose(out, in_, ident), R, W)

    def act(self, out, in_, func, R, W, bias=None, scale=None, accum_out=None):
        kw = {}
        if bias is not None:
            kw["bias"] = bias
        if scale is not None:
            kw["scale"] = scale
        if accum_out is not None:
            kw["accum_out"] = accum_out
        self.op("act", lambda e: e.activation(out=out, in_=in_, func=func, **kw), R, W)

    def tt(self, out, in0, in1, op, R, W, eng="dve"):
        self.op(eng, lambda e: e.tensor_tensor(out=out, in0=in0, in1=in1, op=op), R, W)

    def ts(self, out, in0, s1, s2, op0, op1, R, W, eng="dve", accum_out=None):
        if op1 is None:
            self.op(eng, lambda e: e.tensor_scalar(out=out, in0=in0, scalar1=s1, scalar2=None, op0=op0), R, W)
        elif accum_out is not None:
            self.op(eng, lambda e: e.tensor_scalar(out=out, in0=in0, scalar1=s1, scalar2=s2, op0=op0, op1=op1,
                                                   accum_out=accum_out), R, W)
        else:
            self.op(eng, lambda e: e.tensor_scalar(out=out, in0=in0, scalar1=s1, scalar2=s2, op0=op0, op1=op1), R, W)

    def stt(self, out, in0, scalar, in1, op0, op1, R, W):
        self.op("dve", lambda e: e.scalar_tensor_tensor(out=out, in0=in0, scalar=scalar, in1=in1, op0=op0, op1=op1), R, W)

    def copy(self, out, in_, R, W, eng="dve"):
        if eng == "act":
            self.op("act", lambda e: e.copy(out=out, in_=in_), R, W)
        else:
            self.op(eng, lambda e: e.tensor_copy(out=out, in_=in_), R, W)

    def memset(self, ap, val, W, eng="dve"):
        self.op(eng, lambda e: e.memset(ap, val), (), W)


def host_consts():
    c = {}
    c["ident_f"] = np.eye(128, dtype=np.float32)
    c["ident_b"] = np.eye(128, dtype=np.float32).astype(ml_dtypes.bfloat16)
    return c


CONST_SHAPES = {
    "ident_f": ((128, 128), F32),
    "ident_b": ((128, 128), BF16),
}


class Ctx:
    pass


def load_const(kb, cx, name):
    shape, dt = CONST_SHAPES[name]
    d = kb.inp(name, shape, dt)
    t = kb.tile(shape[1:], dt)
    kb.dma("sp", t[0:shape[0]], d.ap(), R=[kb.tok(name)], W=[t])
    return t


def stage_mods(kb, cx, layers=(0, 1)):
    crows = kb.inp("crows", (2, D))
    ada_w = kb.inp("ada_w", (DEPTH, D, 6 * D))
    ada_b = kb.inp("ada_b", (DEPTH, 6 * D))
    mod_d = kb.dram("mod_d", (DEPTH, 2, 6 * D), F32)
    with kb.scope():
        scT = kb.tile((2, 16))
        for r in range(2):
            kb.dma("sp", scT[:, r, :], crows[r, :].rearrange("(kc p) -> p kc", p=128), R=[kb.tok("crows")], W=[scT],
                   allow_slow_non_contiguous=True)
        kb.act(scT[:], scT[:], AF.Silu, R=[scT], W=[scT])
        wt = [kb.tile((2048,)) for _ in range(3)]
        bt = kb.tile((2048,))
        ot = kb.tile((2048,))
        n = 0
        for l in layers:
            for j in range(6):
                for kc in range(16):
                    w = wt[n % 3]
                    n += 1
                    kb.dma("sp", w[:], ada_w[l, kc * 128:(kc + 1) * 128, j * 2048:(j + 1) * 2048],
                           R=[kb.tok("ada_w")], W=[w])
                    for ct in range(4):
                        kb.mm(kb.bank[ct][0:2, :], scT[:, :, kc], w[:, ct * 512:(ct + 1) * 512],
                              start=(kc == 0), stop=(kc == 15), R=[scT, w], W=[kb.bank[ct]])
                kb.dma("sp", bt[0:2, :], ada_b[l, j * 2048:(j + 1) * 2048].partition_broadcast(2),
                       R=[kb.tok("ada_b")], W=[bt])
                for ct in range(4):
                    kb.tt(ot[0:2, ct * 512:(ct + 1) * 512], kb.bank[ct][0:2, :], bt[0:2, ct * 512:(ct + 1) * 512],
                          ALU.add, R=[kb.bank[ct], bt], W=[ot])
                if j in (1, 4):
                    kb.ts(ot[0:2, :], ot[0:2, :], 1.0, None, ALU.add, None, R=[ot], W=[ot])
                kb.dma("sp", mod_d[l, :, j * 2048:(j + 1) * 2048], ot[0:2, :], R=[ot], W=[kb.tok("mod_d", l, j)])


def load_mod(kb, l, j, r, t):
    mod_d = kb.dram("mod_d", (DEPTH, 2, 6 * D), F32)
    kb.dma("sp", t[:], mod_d[l, r, j * 2048:(j + 1) * 2048].partition_broadcast(128),
           R=[kb.tok("mod_d", l, j)], W=[t])


def layer_norm_tile(kb, xt, rows, st, mv, rstd, W_extra=()):
    for c4 in range(4):
        kb.op("dve", lambda e, c4=c4: e.bn_stats(out=st[0:rows, c4, :], in_=xt[0:rows, c4 * 512:(c4 + 1) * 512]),
              R=[xt], W=[st])
    kb.op("dve", lambda e: e.bn_aggr(out=mv[0:rows, :], in_=st[0:rows, :, :].rearrange("p a b -> p (a b)")), R=[st], W=[mv])
    kb.act(rstd[0:rows, :], mv[0:rows, 1:2], AF.Sqrt, R=[mv, kb.eps], W=[rstd], bias=cx_eps(kb)[0:rows, :], scale=1.0)
    kb.op("dve", lambda e: e.reciprocal(out=rstd[0:rows, :], in_=rstd[0:rows, :]), R=[rstd], W=[rstd])


def cx_eps(kb):
    return kb.eps


def stage_a1(kb, cx, l, src_name, jsh=0, jsc=1, dst="hT_d", tt0=0):
    x_d = kb.inp(src_name, (NTOK, D)) if src_name == "xin" else kb.dram(src_name, (NTOK, D), F32)
    hT_d = kb.dram(dst, (128, 16, NTOK), BF16)
    with kb.scope():
        ident = load_const(kb, cx, "ident_b")
        sh = [kb.tile((2048,)) for _ in range(2)]
        sc = [kb.tile((2048,)) for _ in range(2)]
        for r in range(2):
            load_mod(kb, l, jsh, r, sh[r])
            load_mod(kb, l, jsc, r, sc[r])
        xt = [kb.tile((2048,)) for _ in range(2)]
        st = kb.tile((4, 6))
        mv = kb.tile((2,))
        rstd = kb.tile((1,))
        xn = kb.tile((2048,))
        hb = [kb.tile((2048,), BF16) for _ in range(2)]
        hT = [kb.tile((16, 128), BF16) for _ in range(2)]
        for tt in range(tt0, NTT):
            r = 1 if tt < 2 else 0
            x = xt[tt % 2]
            kb.dma("sp", x[:], x_d[tt * 128:(tt + 1) * 128, :], R=[kb.tok(src_name, tt)], W=[x])
            layer_norm_tile(kb, x, 128, st, mv, rstd)
            kb.ts(xn[:], x[:], mv[:, 0:1], rstd[:, 0:1], ALU.subtract, ALU.mult, R=[x, mv, rstd], W=[xn])
            kb.tt(xn[:], xn[:], sc[r][:], ALU.mult, R=[xn, sc[r]], W=[xn], eng="pool")
            h = hb[tt % 2]
            kb.tt(h[:], xn[:], sh[r][:], ALU.add, R=[xn, sh[r]], W=[h])
            o = hT[tt % 2]
            for half in range(2):
                bk = kb.bank[(tt % 2) * 2 + half]
                pv = bk.ap.bitcast(BF16).rearrange("p (a b) -> p a b", b=128)
                for k8 in range(8):
                    kc = half * 8 + k8
                    kb.tr(pv[:, k8, :], h[:, kc * 128:(kc + 1) * 128], ident[:], R=[h, ident], W=[bk])
                kb.copy(o[:, half * 8:(half + 1) * 8, :], pv[:, :, :], R=[bk], W=[o], eng=("act" if half else "dve"))
            kb.dma("sp", hT_d[:, :, tt * 128:(tt + 1) * 128], o[:], R=[o], W=[kb.tok(dst, tt // 4)])


def stage_a1b(kb, cx, l):
    w_in = kb.inp("w_in", (DEPTH, D, D_IN))
    hT_d = kb.dram("hT_d", (128, 16, NTOK), BF16)
    uT_d = kb.dram("uT_d", (36, 128, NTOK), F32)
    v_d = kb.dram("v_d", (NTOK, 1024), BF16)
    tiles = [(0, 256)] + [(256 + i * 512, 512) for i in range(8)]
    with kb.scope():
        wt = [kb.tile((16, 512), BF16) for _ in range(2)]
        ht = [kb.tile((16, 512), BF16) for _ in range(2)]
        us = [kb.tile((4, 512)) for _ in range(2)]
        vs = [kb.tile((512,), BF16) for _ in range(2)]
        n = 0
        m = 0
        for cg in range(11):
            w = wt[cg % 2]
            for q4 in range(4):
                kb.dma("pool", w[:, q4 * 4:(q4 + 1) * 4, :],
                       w_in[l, q4 * 512:(q4 + 1) * 512, cg * 512:(cg + 1) * 512].rearrange("(kc p) c -> p kc c", p=128),
                       R=[kb.tok("w_in")], W=[w])
            for (t0, nt) in tiles:
                h = ht[n % 2]
                kb.dma("sp", h[:, :, 0:nt], hT_d[:, :, t0:t0 + nt],
                       R=[kb.tok("hT_d", i) for i in range(t0 // 512, (t0 + nt - 1) // 512 + 1)], W=[h])
                if cg < 9:
                    u = us[n % 2]
                    for j in range(4):
                        bk = kb.bank[(n % 2) * 4 + j]
                        for kc in range(16):
                            kb.mm(bk[:, 0:nt], w[:, kc, j * 128:(j + 1) * 128], h[:, kc, 0:nt],
                                  start=(kc == 0), stop=(kc == 15), R=[w, h], W=[bk])
                        kb.copy(u[:, j, 0:nt], bk[:, 0:nt], R=[bk], W=[u], eng=("act" if j % 2 else "dve"))
                    kb.dma("sp", uT_d[cg * 4:(cg + 1) * 4, :, t0:t0 + nt].rearrange("g p t -> p g t"), u[:, :, 0:nt],
                           R=[u], W=[kb.tok("uT_d", cg * 4 + j) for j in range(4)])
                else:
                    for s in range(nt // 128):
                        bk = kb.bank[m % 4]
                        v = vs[m % 2]
                        m += 1
                        for kc in range(16):
                            kb.mm(bk[:, :], h[:, kc, s * 128:(s + 1) * 128], w[:, kc, :],
                                  start=(kc == 0), stop=(kc == 15), R=[w, h], W=[bk])
                        kb.copy(v[:], bk[:, :], R=[bk], W=[v], eng=("act" if m % 2 else "dve"))
                        kb.dma("sp", v_d[t0 + s * 128:t0 + (s + 1) * 128, (cg - 9) * 512:(cg - 8) * 512], v[:],
                               R=[v], W=[kb.tok("v_d")])
                n += 1


def hy_feats(n):
    pos = np.arange(n, dtype=np.float32)
    t = (pos / np.float32(max(n - 1, 1)))[:, None]
    bands = np.linspace(1e-4, 7, 8, dtype=np.float32)
    ang = np.float32(2.0 * math.pi / n) * pos[:, None] * bands
    feats = np.concatenate([t, np.cos(ang), -np.sin(ang)], axis=-1).astype(np.float32)
    return np.ascontiguousarray(feats.T)


def hy_env(n):
    pos = np.arange(n, dtype=np.float32)
    t = (pos / np.float32(max(n - 1, 1)))[:, None]
    mx = math.log(1e-2) / 0.3
    mn = math.log(1e-2) / 1.5
    decay = np.abs(np.linspace(mn, mx, 512, dtype=np.float32))
    env = np.exp(-t * decay).astype(np.float32)
    return np.ascontiguousarray(env.T)


def rope_tables():
    n = NLAT
    rows = n // 64
    row = np.broadcast_to(np.arange(rows, dtype=np.float32)[:, None], (rows, 64)).reshape(-1)
    col = np.broadcast_to(np.arange(64, dtype=np.float32)[None, :], (rows, 64)).reshape(-1)
    inv = (10000.0 ** (-np.arange(0, 64, 2, dtype=np.float32) / 64)).astype(np.float32)
    ang = np.stack([row[:, None] * inv, col[:, None] * inv], axis=1)
    cos = np.cos(ang).astype(np.float32)
    sin = np.sin(ang).astype(np.float32)
    cosT = np.zeros((128, n), np.float32)
    sinT = np.zeros((128, n), np.float32)
    P = np.zeros((128, 128), np.float32)
    for ax in range(2):
        for half in range(2):
            for f in range(32):
                d = ax * 64 + half * 32 + f
                cosT[d] = cos[:, ax, f]
                sinT[d] = sin[:, ax, f]
                if half == 0:
                    P[d + 32, d] = -1.0
                else:
                    P[d - 32, d] = 1.0
    return cosT, sinT, P


def host_consts():
    c = {}
    c["ident_f"] = np.eye(128, dtype=np.float32)
    c["ident_b"] = np.eye(128, dtype=np.float32).astype(ml_dtypes.bfloat16)
    c["ones_f"] = np.ones((128, 128), np.float32)
    c["featsT_4096"] = hy_feats(4096)
    c["featsT_256"] = hy_feats(256)
    c["env_4096"] = hy_env(4096)
    c["env_256"] = hy_env(256)
    cosT, sinT, P = rope_tables()
    c["rope_cos"] = cosT
    c["rope_sin"] = sinT
    c["rope_P"] = P
    return c


CONST_SHAPES.update({
    "ones_f": ((128, 128), F32),
    "rope_P": ((128, 128), F32),
})


def conv_seg(kb, out, src, cw, cb, g, taps, left, off, n, R, W):
    o = out[:, off:off + n]
    kb.ts(o, src[:, off:off + n], cw[:, left, g:g + 1], cb[:, g:g + 1], ALU.mult, ALU.add, R=R, W=W)
    for k in range(taps):
        sft = k - left
        if sft == 0:
            continue
        if sft < 0:
            a = -sft
            kb.stt(out[:, off + a:off + n], src[:, off:off + n - a], cw[:, k, g:g + 1], out[:, off + a:off + n],
                   ALU.mult, ALU.add, R=R + W, W=W)
        else:
            kb.stt(out[:, off:off + n - sft], src[:, off + sft:off + n], cw[:, k, g:g + 1], out[:, off:off + n - sft],
                   ALU.mult, ALU.add, R=R + W, W=W)


def stage_hyena(kb, cx, l, groups=(0, 1, 2, 3), with_ctx=True):
    uT_d = kb.dram("uT_d", (36, 128, NTOK), F32)
    yT_d = kb.dram("yT_d", (16, 128, NTOK), BF16)
    cwd = kb.inp("hy_conv_w", (DEPTH, 3, 1536))
    cbd = kb.inp("hy_conv_b", (DEPTH, 1536))
    w1d = kb.inp("hy_w1", (DEPTH, 17, 64))
    b1d = kb.inp("hy_b1", (DEPTH, 64))
    w2d = kb.inp("hy_w2", (DEPTH, 64, 64))
    b2d = kb.inp("hy_b2", (DEPTH, 64))
    w3d = kb.inp("hy_w3", (DEPTH, 64, 2048))
    frd = kb.inp("hy_freq", (DEPTH, 64))
    skd = kb.inp("hy_skip", (DEPTH, 2, 512))
    segs = ([(0, NCTX)] if with_ctx else []) + [(NCTX, NLAT)]
    with kb.scope():
        cw = kb.tile((3, 12))
        cb = kb.tile((12,))
        sk = kb.tile((2, 4))
        for k in range(3):
            kb.dma("sp", cw[:, k, :], cwd[l, k, :].rearrange("(g p) -> p g", p=128), R=[kb.tok("hyp")], W=[cw],
                   allow_slow_non_contiguous=True)
        kb.dma("sp", cb[:], cbd[l, :].rearrange("(g p) -> p g", p=128), R=[kb.tok("hyp")], W=[cb],
               allow_slow_non_contiguous=True)
        for o in range(2):
            kb.dma("sp", sk[:, o, :], skd[l, o, :].rearrange("(g p) -> p g", p=128), R=[kb.tok("hyp")], W=[sk],
                   allow_slow_non_contiguous=True)
        w1 = kb.tile((64,))
        w2 = kb.tile((64,))
        w3 = kb.tile((2048,))
        fr = kb.tile((1,))
        fb1 = kb.tile((1,))
        fb2 = kb.tile((1,))
        kb.dma("sp", w1[0:17, :], w1d[l], R=[kb.tok("hyp")], W=[w1])
        kb.dma("sp", w2[0:64, :], w2d[l], R=[kb.tok("hyp")], W=[w2])
        kb.dma("sp", w3[0:64, :], w3d[l], R=[kb.tok("hyp")], W=[w3])
        kb.dma("sp", fr[0:64, :], frd[l, :].rearrange("(p o) -> p o", o=1), R=[kb.tok("hyp")], W=[fr])
        kb.dma("sp", fb1[0:64, :], b1d[l, :].rearrange("(p o) -> p o", o=1), R=[kb.tok("hyp")], W=[fb1])
        kb.dma("sp", fb2[0:64, :], b2d[l, :].rearrange("(p o) -> p o", o=1), R=[kb.tok("hyp")], W=[fb2])
        kb.tt(fb1[0:64, :], fb1[0:64, :], fr[0:64, :], ALU.mult, R=[fb1, fr], W=[fb1])
        kb.tt(fb2[0:64, :], fb2[0:64, :], fr[0:64, :], ALU.mult, R=[fb2, fr], W=[fb2])
        for (off, n) in segs:
            with kb.scope():
                hid2 = kb.tile((n,))
                negpi = kb.tile((1,))
                kb.memset(negpi[:], -math.pi, W=[negpi])
                with kb.scope():
                    ft = kb.tile((n,))
                    fd = kb.inp(f"featsT_{n}", (17, n))
                    kb.dma("sp", ft[0:17, :], fd.ap(), R=[kb.tok("hyp")], W=[ft])
                    hid1 = kb.tile((n,))
                    rri = kb.tile((512,), I32)
                    rrf = kb.tile((512,))
                    tl = min(512, n)
                    for (src, wt, kk, fb, dst) in ((ft, w1, 17, fb1, hid1), (hid1, w2, 64, fb2, hid2)):
                        for i in range(n // tl):
                            bk = kb.bank[i % 4]
                            kb.mm(bk[0:64, 0:tl], wt[0:kk, 0:64], src[0:kk, i * tl:(i + 1) * tl], True, True, R=[wt, src], W=[bk])
                            d = dst[0:64, i * tl:(i + 1) * tl]
                            kb.ts(d, bk[0:64, 0:tl], fr[0:64, 0:1], fb[0:64, 0:1], ALU.mult, ALU.add, R=[bk, fr, fb], W=[dst])
                            yi = rri[0:64, 0:tl]
                            yf = rrf[0:64, 0:tl]
                            kb.ts(d, d, 1.0 / (2.0 * math.pi), 16.5, ALU.mult, ALU.add, R=[dst], W=[dst])
                            kb.copy(yi, d, R=[dst], W=[rri])
                            kb.copy(yf, yi, R=[rri], W=[rrf])
                            kb.tt(d, d, yf, ALU.subtract, R=[dst, rrf], W=[dst])
                            kb.ts(yf, d, 0.0, None, ALU.is_lt, None, R=[dst], W=[rrf])
                            kb.tt(d, d, yf, ALU.add, R=[dst, rrf], W=[dst])
                            kb.act(d, d, AF.Sin, R=[dst, negpi], W=[dst], bias=negpi[0:64, :], scale=2.0 * math.pi)
                envd = kb.inp(f"env_{n}", (512, n))
                env = kb.tile((n,))
                raw = kb.tile((n,))
                z = kb.tile((n,))
                gate = kb.tile((n,))
                fwd = kb.tile((n,))
                bwd = kb.tile((n,))
                acc = kb.tile((n,))
                accx = [kb.tile((n,)) for _ in range(3 if n <= 512 else 1)]
                nrm = kb.tile((4,))
                yb = kb.tile((n,), BF16)
                for g in groups:
                    kb.dma("sp", env[:], envd[g * 128:(g + 1) * 128, :], R=[kb.tok("hyp")], W=[env])
                    kb.dma("sp", raw[:], uT_d[8 + g, :, off:off + n], R=[kb.tok("uT_d", 8 + g)], W=[raw])
                    conv_seg(kb, z, raw, cw, cb, 8 + g, 3, 1, 0, n, R=[raw, cw, cb], W=[z])
                    for o in range(2):
                        kb.dma("sp", raw[:], uT_d[4 * o + g, :, off:off + n], R=[kb.tok("uT_d", 4 * o + g)], W=[raw])
                        conv_seg(kb, gate, raw, cw, cb, 4 * o + g, 3, 1, 0, n, R=[raw, cw, cb], W=[gate])
                        for s_, dst in ((0, fwd), (1, bwd)):
                            c0 = s_ * 1024 + o * 512 + g * 128
                            for i in range(n // tl):
                                bk = kb.bank[4 + i % 4]
                                kb.mm(bk[:, 0:tl], w3[0:64, c0:c0 + 128], hid2[0:64, i * tl:(i + 1) * tl], True, True,
                                      R=[w3, hid2], W=[bk])
                                kb.tt(dst[:, i * tl:(i + 1) * tl], bk[:, 0:tl], env[:, i * tl:(i + 1) * tl], ALU.mult,
                                      R=[bk, env], W=[dst])
                        kb.memset(bwd[:, 0:1], 0.0, W=[bwd])
                        kb.act(acc[:], fwd[:], AF.Abs, R=[fwd], W=[acc])
                        kb.op("dve", lambda e: e.reduce_sum(out=nrm[:, 0:1], in_=acc[:], axis=AX.X), R=[acc], W=[nrm])
                        kb.act(acc[:], bwd[:], AF.Abs, R=[bwd], W=[acc])
                        kb.op("dve", lambda e: e.reduce_sum(out=nrm[:, 1:2], in_=acc[:], axis=AX.X), R=[acc], W=[nrm])
                        kb.tt(nrm[:, 2:3], nrm[:, 0:1], nrm[:, 1:2], ALU.add, R=[nrm], W=[nrm])
                        kb.op("dve", lambda e: e.reciprocal(out=nrm[:, 3:4], in_=nrm[:, 2:3]), R=[nrm], W=[nrm])
                        kb.ts(acc[:], z[:], fwd[:, 0:1], None, ALU.mult, None, R=[z, fwd], W=[acc])
                        for a_ in accx:
                            kb.memset(a_[:], 0.0, W=[a_], eng="pool")
                        accs = [acc] + accx
                        na = len(accs)
                        k_ = 0
                        for dlt in range(1, n):
                            a_ = accs[k_ % na]
                            k_ += 1
                            kb.stt(a_[:, dlt:n], z[:, 0:n - dlt], fwd[:, dlt:dlt + 1], a_[:, dlt:n], ALU.mult, ALU.add,
                                   R=[z, fwd, a_], W=[a_])
                            a_ = accs[k_ % na]
                            k_ += 1
                            kb.stt(a_[:, 0:n - dlt], z[:, dlt:n], bwd[:, dlt:dlt + 1], a_[:, 0:n - dlt], ALU.mult, ALU.add,
                                   R=[z, bwd, a_], W=[a_])
                        for a_ in accx:
                            kb.tt(acc[:], acc[:], a_[:], ALU.add, R=[acc, a_], W=[acc])
                        kb.checkpoint()
                        kb.ts(acc[:], acc[:], nrm[:, 3:4], None, ALU.mult, None, R=[acc, nrm], W=[acc])
                        kb.stt(acc[:], z[:], sk[:, o, g:g + 1], acc[:], ALU.mult, ALU.add, R=[z, sk, acc], W=[acc])
                        kb.tt(z[:], acc[:], gate[:], ALU.mult, R=[acc, gate], W=[z])
                    kb.copy(yb[:], z[:], R=[z], W=[yb], eng="act")
                    kb.dma("sp", yT_d[g, :, off:off + n], yb[:], R=[yb], W=[kb.tok("yT_d", g)])


def stage_lru(kb, cx, l, groups=(0, 1, 2, 3)):
    uT_d = kb.dram("uT_d", (36, 128, NTOK), F32)
    yT_d = kb.dram("yT_d", (16, 128, NTOK), BF16)
    cwd = kb.inp("lru_conv_w", (DEPTH, 4, 512))
    cbd = kb.inp("lru_conv_b", (DEPTH, 512))
    wad = kb.inp("lru_wa", (DEPTH, 2, 8, 64, 64))
    bad = kb.inp("lru_ba", (DEPTH, 2, 512))
    wxd = kb.inp("lru_wx", (DEPTH, 2, 8, 64, 64))
    bxd = kb.inp("lru_bx", (DEPTH, 2, 512))
    lmd = kb.inp("lru_lambda", (DEPTH, 2, 512))
    N = NTOK
    with kb.scope():
        cw = kb.tile((4, 4))
        cb = kb.tile((4,))
        for k in range(4):
            kb.dma("sp", cw[:, k, :], cwd[l, k, :].rearrange("(g p) -> p g", p=128), R=[kb.tok("lrp")], W=[cw],
                   allow_slow_non_contiguous=True)
        kb.dma("sp", cb[:], cbd[l, :].rearrange("(g p) -> p g", p=128), R=[kb.tok("lrp")], W=[cb], allow_slow_non_contiguous=True)
        bia = kb.tile((3, 2, 4))
        for i, src in enumerate((bad, bxd, lmd)):
            for d in range(2):
                kb.dma("sp", bia[:, i, d, :], src[l, d, :].rearrange("(g p) -> p g", p=128), R=[kb.tok("lrp")], W=[bia],
                       allow_slow_non_contiguous=True)
        sp_ = kb.tile((2, 4))
        n8 = kb.tile((2, 4))
        n16 = kb.tile((2, 4))
        kb.act(sp_[:], bia[:, 2, :, :], AF.Exp, R=[bia], W=[sp_], scale=-1.0)
        kb.act(sp_[:], sp_[:], AF.Ln, R=[sp_], W=[sp_], bias=1.0, scale=1.0)
        kb.ts(n8[:], sp_[:], -8.0, None, ALU.mult, None, R=[sp_], W=[n8])
        kb.ts(n16[:], sp_[:], -16.0, None, ALU.mult, None, R=[sp_], W=[n16])
        wbd = kb.tile((2, 2, 128))
        raw = kb.tile((N,))
        xr = kb.tile((N,))
        gt = kb.tile((N,))
        rr = kb.tile((N,))
        ii = kb.tile((N,))
        aa = kb.tile((N,))
        hh = kb.tile((N,))
        h2 = kb.tile((N,))
        yb = kb.tile((N,), BF16)
        tiles = [(0, 256)] + [(256 + i * 512, 512) for i in range(8)]
        for g in groups:
            kb.memset(wbd[:], 0.0, W=[wbd])
            for i, src in enumerate((wad, wxd)):
                for d in range(2):
                    for hh_ in range(2):
                        kb.dma("sp", wbd[hh_ * 64:(hh_ + 1) * 64, i, d, hh_ * 64:(hh_ + 1) * 64], src[l, d, 2 * g + hh_],
                               R=[kb.tok("lrp")], W=[wbd])
            kb.dma("sp", raw[:], uT_d[16 + g, :, :], R=[kb.tok("uT_d", 16 + g)], W=[raw])
            kb.dma("sp", gt[:], uT_d[12 + g, :, :], R=[kb.tok("uT_d", 12 + g)], W=[gt])
            for (off, n) in ((0, NCTX), (NCTX, NLAT)):
                conv_seg(kb, xr, raw, cw, cb, g, 4, 2, off, n, R=[raw, cw, cb], W=[xr])
            for d in range(2):
                for i, dst in ((0, rr), (1, ii)):
                    for ti, (t0, nt) in enumerate(tiles):
                        bk = kb.bank[ti % 4 + 4 * i]
                        kb.mm(bk[:, 0:nt], wbd[:, i, d, :], xr[:, t0:t0 + nt], True, True, R=[wbd, xr], W=[bk])
                        kb.act(dst[:, t0:t0 + nt], bk[:, 0:nt], AF.Sigmoid, R=[bk, bia], W=[dst], bias=bia[:, i, d, g:g + 1], scale=1.0)
                kb.act(aa[:], rr[:], AF.Exp, R=[rr, n8], W=[aa], scale=n8[:, d, g:g + 1])
                kb.act(rr[:], rr[:], AF.Exp, R=[rr, n16], W=[rr], scale=n16[:, d, g:g + 1])
                kb.act(rr[:], rr[:], AF.Sqrt, R=[rr], W=[rr], bias=1.0, scale=-1.0)
                kb.tt(ii[:], ii[:], xr[:], ALU.mult, R=[ii, xr], W=[ii])
                kb.tt(ii[:], ii[:], rr[:], ALU.mult, R=[ii, rr], W=[ii], eng="pool")
                dsth = hh if d == 0 else h2
                if d == 0:
                    kb.op("dve", lambda e: e.tensor_tensor_scan(out=hh[:], data0=aa[:], data1=ii[:], initial=0.0,
                                                                op0=ALU.mult, op1=ALU.add), R=[aa, ii], W=[hh])
                else:
                    kb.op("dve", lambda e: e.tensor_tensor_scan(out=h2[:, NCTX - 1::-1], data0=aa[:, NCTX - 1::-1],
                                                                data1=ii[:, NCTX - 1::-1], initial=0.0,
                                                                op0=ALU.mult, op1=ALU.add), R=[aa, ii], W=[h2])
                    kb.op("dve", lambda e: e.tensor_tensor_scan(out=h2[:, N - 1:NCTX - 1:-1], data0=aa[:, N - 1:NCTX - 1:-1],
                                                                data1=ii[:, N - 1:NCTX - 1:-1], initial=h2[:, 0:1],
                                                                op0=ALU.mult, op1=ALU.add), R=[aa, ii, h2], W=[h2])
            kb.tt(hh[:], hh[:], h2[:], ALU.add, R=[hh, h2], W=[hh])
            kb.tt(rr[:], gt[:], gt[:], ALU.mult, R=[gt], W=[rr], eng="pool")
            kb.ts(rr[:], rr[:], 0.044715, 1.0, ALU.mult, ALU.add, R=[rr], W=[rr])
            kb.tt(rr[:], rr[:], gt[:], ALU.mult, R=[rr, gt], W=[rr], eng="pool")
            kb.act(rr[:], rr[:], AF.Sigmoid, R=[rr], W=[rr], scale=1.5957691216057308)
            kb.tt(rr[:], rr[:], gt[:], ALU.mult, R=[rr, gt], W=[rr])
            kb.tt(yb[:], rr[:], hh[:], ALU.mult, R=[rr, hh], W=[yb])
            kb.dma("sp", yT_d[4 + g, :, :], yb[:], R=[yb], W=[kb.tok("yT_d", 4 + g)])


def stage_att(kb, cx, l, heads=(0, 1, 2, 3), with_ctx=True):
    uT_d = kb.dram("uT_d", (36, 128, NTOK), F32)
    v_d = kb.dram("v_d", (NTOK, 1024), BF16)
    yT_d = kb.dram("yT_d", (16, 128, NTOK), BF16)
    lvd = kb.inp("att_lambda", (DEPTH, 4, 128))
    sgd = kb.inp("att_subln", (DEPTH, 256))
    cosd = kb.inp("rope_cos", (128, NLAT))
    sind = kb.inp("rope_sin", (128, NLAT))
    lam_init = 0.8 - 0.6 * math.exp(-0.3 * l)
    with kb.scope():
        ident = load_const(kb, cx, "ident_b")
        ones = load_const(kb, cx, "ones_f")
        P = load_const(kb, cx, "rope_P")
        cos = kb.tile((NLAT,))
        sin = kb.tile((NLAT,))
        kb.dma("sp", cos[:], cosd.ap(), R=[kb.tok("attp")], W=[cos])
        kb.dma("sp", sin[:], sind.ap(), R=[kb.tok("attp")], W=[sin])
        lv = kb.tile((4,))
        kb.dma("sp", lv[:], lvd[l].rearrange("r d -> d r"), R=[kb.tok("attp")], W=[lv], allow_slow_non_contiguous=True)
        pr = kb.tile((2,))
        kb.tt(pr[:, 0:1], lv[:, 0:1], lv[:, 1:2], ALU.mult, R=[lv], W=[pr])
        kb.tt(pr[:, 1:2], lv[:, 2:3], lv[:, 3:4], ALU.mult, R=[lv], W=[pr])
        kb.mm(kb.bank[7][:, 0:2], ones[:], pr[:, 0:2], True, True, R=[ones, pr], W=[kb.bank[7]])
        lam = kb.tile((2,))
        kb.act(lam[:], kb.bank[7][:, 0:2], AF.Exp, R=[kb.bank[7]], W=[lam])
        nlam = kb.tile((1,))
        kb.tt(nlam[:], lam[:, 1:2], lam[:, 0:1], ALU.subtract, R=[lam], W=[nlam])
        kb.ts(nlam[:], nlam[:], -lam_init, None, ALU.add, None, R=[nlam], W=[nlam])
        gain = kb.tile((256,))
        kb.dma("sp", gain[:], sgd[l, :].partition_broadcast(128), R=[kb.tok("attp")], W=[gain])
        kb.ts(gain[:], gain[:], 1.0 - lam_init, None, ALU.mult, None, R=[gain], W=[gain])
        raw = kb.tile((NTOK,))
        rot = kb.tile((NLAT,))
        qT = [kb.tile((NTOK,), BF16) for _ in range(2)]
        kT = [kb.tile((NTOK,), BF16) for _ in range(2)]
        vaug = kb.tile((NTT, 257), BF16)
        et = [kb.tile((256,), BF16) for _ in range(4)]
        r12 = kb.tile((4,))
        osb = kb.tile((256,))
        sq = kb.tile((256,))
        ss = kb.tile((1,))
        ob = kb.tile((256,), BF16)
        yTt = [kb.tile((2, 128), BF16) for _ in range(2)]
        cnt = 0
        nout = 0
        for hd in heads:
            for m in range(2):
                for (grp, dst) in ((20 + 2 * hd + m, qT[m]), (28 + 2 * hd + m, kT[m])):
                    kb.dma("sp", raw[:], uT_d[grp, :, :], R=[kb.tok("uT_d", grp)], W=[raw])
                    for i in range(8):
                        bk = kb.bank[4 + i % 3]
                        kb.mm(bk[:, :], P[:], raw[:, NCTX + i * 512:NCTX + (i + 1) * 512], True, True, R=[P, raw], W=[bk])
                        kb.tt(rot[:, i * 512:(i + 1) * 512], bk[:, :], sin[:, i * 512:(i + 1) * 512], ALU.mult, R=[bk, sin], W=[rot])
                    kb.copy(dst[:, 0:NCTX], raw[:, 0:NCTX], R=[raw], W=[dst], eng="act")
                    kb.tt(raw[:, NCTX:], raw[:, NCTX:], cos[:], ALU.mult, R=[raw, cos], W=[raw], eng="pool")
                    kb.tt(dst[:, NCTX:], raw[:, NCTX:], rot[:], ALU.add, R=[raw, rot], W=[dst])
            kb.dma("sp", vaug[:, :, 0:256], v_d[:, hd * 256:(hd + 1) * 256].rearrange("(c p) e -> p c e", p=128),
                   R=[kb.tok("v_d")], W=[vaug])
            kb.memset(vaug[:, :, 256:257], 1.0, W=[vaug])
            qtiles = ([(0, 2)] if with_ctx else []) + [(NCTX + i * 256, NTT) for i in range(16)]
            for (q0, nkc) in qtiles:
                for kc in range(nkc):
                    for m in range(2):
                        sb = kb.bank[4 + cnt % 3]
                        e_ = et[cnt % 4]
                        cnt += 1
                        kb.mm(sb[:, 0:256], kT[m][:, kc * 128:(kc + 1) * 128], qT[m][:, q0:q0 + 256], True, True,
                              R=[kT[m], qT[m]], W=[sb])
                        kb.act(e_[:], sb[:, 0:256], AF.Exp, R=[sb], W=[e_], scale=128.0 ** -0.5)
                        for qb in range(2):
                            ob_ = kb.bank[m * 2 + qb]
                            kb.mm(ob_[:, 0:257], e_[:, qb * 128:(qb + 1) * 128], vaug[:, kc, :], (kc == 0), (kc == nkc - 1),
                                  R=[e_, vaug], W=[ob_])
                for qb in range(2):
                    O1 = kb.bank[qb]
                    O2 = kb.bank[2 + qb]
                    kb.op("dve", lambda e, O1=O1: e.reciprocal(out=r12[:, 0:1], in_=O1[:, 256:257]), R=[O1], W=[r12])
                    kb.op("dve", lambda e, O2=O2: e.reciprocal(out=r12[:, 1:2], in_=O2[:, 256:257]), R=[O2], W=[r12])
                    kb.tt(r12[:, 2:3], r12[:, 1:2], nlam[:, 0:1], ALU.mult, R=[r12, nlam], W=[r12])
                    kb.ts(osb[:], O1[:, 0:256], r12[:, 0:1], None, ALU.mult, None, R=[O1, r12], W=[osb])
                    kb.stt(osb[:], O2[:, 0:256], r12[:, 2:3], osb[:], ALU.mult, ALU.add, R=[O2, r12, osb], W=[osb])
                    kb.tt(sq[:], osb[:], osb[:], ALU.mult, R=[osb], W=[sq], eng="pool")
                    kb.op("dve", lambda e: e.reduce_sum(out=ss[:, 0:1], in_=sq[:], axis=AX.X), R=[sq], W=[ss])
                    kb.act(ss[:], ss[:], AF.Sqrt, R=[ss, kb.eps], W=[ss], bias=kb.eps[:, 0:1], scale=1.0 / 256.0)
                    kb.op("dve", lambda e: e.reciprocal(out=ss[:], in_=ss[:]), R=[ss], W=[ss])
                    kb.ts(osb[:], osb[:], ss[:, 0:1], None, ALU.mult, None, R=[osb, ss], W=[osb])
                    kb.tt(ob[:], osb[:], gain[:], ALU.mult, R=[osb, gain], W=[ob])
                    bk7 = kb.bank[7]
                    pv = bk7.ap.bitcast(BF16).rearrange("p (a b) -> p a b", b=128)
                    yt_ = yTt[nout % 2]
                    nout += 1
                    for j in range(2):
                        kb.tr(pv[:, j, :], ob[:, j * 128:(j + 1) * 128], ident[:], R=[ob, ident], W=[bk7])
                    kb.copy(yt_[:], pv[:, 0:2, :], R=[bk7], W=[yt_], eng="act")
                    t0 = q0 + qb * 128
                    kb.dma("sp", yT_d[8 + 2 * hd:10 + 2 * hd, :, t0:t0 + 128].rearrange("j p t -> p j t"), yt_[:],
                           R=[yt_], W=[kb.tok("yT_d", 8 + 2 * hd), kb.tok("yT_d", 9 + 2 * hd)])


def stage_b1(kb, cx, l, src_name, tt0):
    x_d = kb.inp(src_name, (NTOK, D)) if src_name == "xin" else kb.dram(src_name, (NTOK, D), F32)
    yT_d = kb.dram("yT_d", (16, 128, NTOK), BF16)
    x1_d = kb.dram("x1_d", (NTOK, D), F32)
    w_out = kb.inp("w_out", (DEPTH, D, D))
    lng = kb.inp("ln_g", (DEPTH, 2, D))
    lnb = kb.inp("ln_b", (DEPTH, 2, D))
    with kb.scope():
        wo = kb.tile((16, 2048), BF16)
        for kc in range(16):
            kb.dma("pool", wo[:, kc, :], w_out[l, kc * 128:(kc + 1) * 128, :], R=[kb.tok("w_out")], W=[wo])
        g1 = [kb.tile((2048,)) for _ in range(2)]
        for r in range(2):
            load_mod(kb, l, 2, r, g1[r])
        gg = kb.tile((2048,))
        bb = kb.tile((2048,))
        kb.dma("sp", gg[:], lng[l, 0, :].partition_broadcast(128), R=[kb.tok("lnp")], W=[gg])
        kb.dma("sp", bb[:], lnb[l, 0, :].partition_broadcast(128), R=[kb.tok("lnp")], W=[bb])
        yt = [kb.tile((16, 128), BF16) for _ in range(2)]
        xt = [kb.tile((2048,)) for _ in range(2)]
        xo = [kb.tile((2048,)) for _ in range(2)]
        t = kb.tile((2048,))
        st = kb.tile((4, 6))
        mv = kb.tile((2,))
        rstd = kb.tile((1,))
        for tt in range(tt0, NTT):
            r = 1 if tt < 2 else 0
            y = yt[tt % 2]
            x = xt[tt % 2]
            kb.dma("sp", y[:], yT_d[:, :, tt * 128:(tt + 1) * 128].rearrange("kc p t -> p kc t"),
                   R=[kb.tok("yT_d", i) for i in range(16)], W=[y])
            kb.dma("sp", x[:], x_d[tt * 128:(tt + 1) * 128, :], R=[kb.tok(src_name, tt)], W=[x])
            for ct in range(4):
                bk = kb.bank[(tt % 2) * 4 + ct]
                for kc in range(16):
                    kb.mm(bk[:, :], y[:, kc, :], wo[:, kc, ct * 512:(ct + 1) * 512], (kc == 0), (kc == 15), R=[y, wo], W=[bk])
                kb.tt(t[:, ct * 512:(ct + 1) * 512], bk[:, :], g1[r][:, ct * 512:(ct + 1) * 512], ALU.mult, R=[bk, g1[r]], W=[t])
            kb.stt(t[:], x[:], ALPHA, t[:], ALU.mult, ALU.add, R=[x, t], W=[t])
            layer_norm_tile(kb, t, 128, st, mv, rstd)
            kb.ts(t[:], t[:], mv[:, 0:1], rstd[:, 0:1], ALU.subtract, ALU.mult, R=[t, mv, rstd], W=[t])
            kb.tt(t[:], t[:], gg[:], ALU.mult, R=[t, gg], W=[t], eng="pool")
            o = xo[tt % 2]
            kb.tt(o[:], t[:], bb[:], ALU.add, R=[t, bb], W=[o])
            kb.dma("sp", x1_d[tt * 128:(tt + 1) * 128, :], o[:], R=[o], W=[kb.tok("x1_d", tt)])


def stage_moe(kb, cx, l, tt0, final, experts=65):
    fT_d = kb.dram("fT_d", (128, 16, NTOK), BF16)
    x1_d = kb.dram("x1_d", (NTOK, D), F32)
    if final:
        kb.io_out.add("out")
        dst_d = kb.dram("out", (NLAT, D), F32)
    else:
        dst_d = kb.dram("xres", (NTOK, D), F32)
    rw = kb.inp("router_w", (DEPTH, D, NEXP))
    rb = kb.inp("router_b", (DEPTH, NEXP))
    wg = kb.inp("exp_w_gate", (DEPTH, NEXP, D, DEXP))
    wu = kb.inp("exp_w_up", (DEPTH, NEXP, D, DEXP))
    wd = kb.inp("exp_w_down", (DEPTH, NEXP, DEXP, D))
    sg = kb.inp("sh_w_gate", (DEPTH, D, DEXP))
    su = kb.inp("sh_w_up", (DEPTH, D, DEXP))
    sd = kb.inp("sh_w_down", (DEPTH, DEXP, D))
    lng = kb.inp("ln_g", (DEPTH, 2, D))
    lnb = kb.inp("ln_b", (DEPTH, 2, D))
    with kb.scope():
        ident = load_const(kb, cx, "ident_b")
        G = kb.tile((NTT, 65))
        with kb.scope():
            rwt = kb.tile((16, 64), BF16)
            kb.dma("pool", rwt[:], rw[l].rearrange("(kc p) e -> p kc e", p=128), R=[kb.tok("rw")], W=[rwt])
            rbt = kb.tile((64,))
            kb.dma("sp", rbt[:], rb[l, :].partition_broadcast(128), R=[kb.tok("rw")], W=[rbt])
            ft = [kb.tile((16, 128), BF16) for _ in range(2)]
            sc = kb.tile((64,))
            sbb = kb.tile((64,))
            top = kb.tile((8,))
            den = kb.tile((2,))
            for tt in range(tt0, NTT):
                f = ft[tt % 2]
                kb.dma("sp", f[:], fT_d[:, :, tt * 128:(tt + 1) * 128], R=[kb.tok("fT_d", tt // 4)], W=[f])
                bk = kb.bank[tt % 2]
                for kc in range(16):
                    kb.mm(bk[:, 0:64], f[:, kc, :], rwt[:, kc, :], (kc == 0), (kc == 15), R=[f, rwt], W=[bk])
                kb.act(sc[:], bk[:, 0:64], AF.Sigmoid, R=[bk], W=[sc])
                kb.tt(sbb[:], sc[:], rbt[:], ALU.add, R=[sc, rbt], W=[sbb])
                kb.op("dve", lambda e: e.max(out=top[:], in_=sbb[:]), R=[sbb], W=[top])
                kb.ts(sbb[:], sbb[:], top[:, 7:8], None, ALU.is_ge, None, R=[sbb, top], W=[sbb])
                kb.tt(sbb[:], sbb[:], sc[:], ALU.mult, R=[sbb, sc], W=[sbb])
                kb.op("dve", lambda e: e.reduce_sum(out=den[:, 0:1], in_=sbb[:], axis=AX.X), R=[sbb], W=[den])
                kb.op("dve", lambda e: e.reciprocal(out=den[:, 1:2], in_=den[:, 0:1]), R=[den], W=[den])
                kb.ts(G[:, tt, 0:64], sbb[:], den[:, 1:2], 2.5, ALU.mult, ALU.mult, R=[sbb, den], W=[G])
                kb.memset(G[:, tt, 64:65], 1.0, W=[G])
        acc = [kb.tile((2048,)) for _ in range(8)]
        for c0 in range(tt0, NTT, 8):
            tiles = list(range(c0, min(c0 + 8, NTT)))
            with kb.scope():
                wgt = kb.tile((16, 512), BF16)
                wut = kb.tile((16, 512), BF16)
                wdt = kb.tile((4, 2048), BF16)
                stg = [kb.tile((4, 512)) for _ in range(2)]
                ft = [kb.tile((16, 128), BF16) for _ in range(2)]
                a_sb = kb.tile((512,))
                ab = [kb.tile((512,), BF16) for _ in range(2)]
                aT = [kb.tile((4, 128), BF16) for _ in range(2)]
                ns = 0
                n = 0
                for e in range(experts):
                    kb.checkpoint()
                    gsrc = wg[l, e] if e < 64 else sg[l]
                    usrc = wu[l, e] if e < 64 else su[l]
                    dsrc = wd[l, e] if e < 64 else sd[l]
                    for (src, dstw) in ((gsrc, wgt), (usrc, wut)):
                        for q4 in range(4):
                            s_ = stg[ns % 2]
                            ns += 1
                            kb.dma("sp", s_[:], src[q4 * 512:(q4 + 1) * 512, :].rearrange("(kc p) c -> p kc c", p=128),
                                   R=[kb.tok("expw")], W=[s_])
                            kb.copy(dstw[:, q4 * 4:(q4 + 1) * 4, :], s_[:], R=[s_], W=[dstw], eng="pool")
                    for k4 in range(4):
                        s_ = stg[ns % 2]
                        ns += 1
                        kb.dma("sp", s_[:].rearrange("p a b -> p (a b)"), dsrc[k4 * 128:(k4 + 1) * 128, :], R=[kb.tok("expw")], W=[s_])
                        kb.copy(wdt[:, k4, :], s_[:].rearrange("p a b -> p (a b)"), R=[s_], W=[wdt], eng="pool")
                    for tt in tiles:
                        f = ft[n % 2]
                        a_b = ab[n % 2]
                        a_t = aT[n % 2]
                        n += 1
                        kb.dma("sp", f[:], fT_d[:, :, tt * 128:(tt + 1) * 128], R=[kb.tok("fT_d", tt // 4)], W=[f])
                        for kc in range(16):
                            kb.mm(kb.bank[0][:, :], f[:, kc, :], wgt[:, kc, :], (kc == 0), (kc == 15), R=[f, wgt], W=[kb.bank[0]])
                        for kc in range(16):
                            kb.mm(kb.bank[1][:, :], f[:, kc, :], wut[:, kc, :], (kc == 0), (kc == 15), R=[f, wut], W=[kb.bank[1]])
                        kb.act(a_sb[:], kb.bank[0][:, :], AF.Silu, R=[kb.bank[0]], W=[a_sb])
                        kb.stt(a_b[:], a_sb[:], G[:, tt, e:e + 1], kb.bank[1][:, :], ALU.mult, ALU.mult, R=[a_sb, G, kb.bank[1]], W=[a_b])
                        b2 = kb.bank[2]
                        pv = b2.ap.bitcast(BF16).rearrange("p (a b) -> p a b", b=128)
                        for k4 in range(4):
                            kb.tr(pv[:, k4, :], a_b[:, k4 * 128:(k4 + 1) * 128], ident[:], R=[a_b, ident], W=[b2])
                        kb.copy(a_t[:], pv[:, 0:4, :], R=[b2], W=[a_t], eng="act")
                        a = acc[tt - c0]
                        for ct in range(4):
                            bk = kb.bank[3 + ct]
                            for k4 in range(4):
                                kb.mm(bk[:, :], a_t[:, k4, :], wdt[:, k4, ct * 512:(ct + 1) * 512], (k4 == 0), (k4 == 3), R=[a_t, wdt], W=[bk])
                            if e == 0:
                                kb.copy(a[:, ct * 512:(ct + 1) * 512], bk[:, :], R=[bk], W=[a])
                            else:
                                kb.tt(a[:, ct * 512:(ct + 1) * 512], a[:, ct * 512:(ct + 1) * 512], bk[:, :], ALU.add, R=[a, bk], W=[a])
            with kb.scope():
                g2 = [kb.tile((2048,)) for _ in range(2)]
                for r in range(2):
                    load_mod(kb, l, 5, r, g2[r])
                gg = kb.tile((2048,))
                bb = kb.tile((2048,))
                kb.dma("sp", gg[:], lng[l, 1, :].partition_broadcast(128), R=[kb.tok("lnp")], W=[gg])
                kb.dma("sp", bb[:], lnb[l, 1, :].partition_broadcast(128), R=[kb.tok("lnp")], W=[bb])
                xt = [kb.tile((2048,)) for _ in range(2)]
                st = kb.tile((4, 6))
                mv = kb.tile((2,))
                rstd = kb.tile((1,))
                for tt in tiles:
                    r = 1 if tt < 2 else 0
                    x = xt[tt % 2]
                    a = acc[tt - c0]
                    kb.dma("sp", x[:], x1_d[tt * 128:(tt + 1) * 128, :], R=[kb.tok("x1_d", tt)], W=[x])
                    kb.tt(a[:], a[:], g2[r][:], ALU.mult, R=[a, g2[r]], W=[a])
                    kb.stt(a[:], x[:], ALPHA, a[:], ALU.mult, ALU.add, R=[x, a], W=[a])
                    layer_norm_tile(kb, a, 128, st, mv, rstd)
                    kb.ts(a[:], a[:], mv[:, 0:1], rstd[:, 0:1], ALU.subtract, ALU.mult, R=[a, mv, rstd], W=[a])
                    kb.tt(a[:], a[:], gg[:], ALU.mult, R=[a, gg], W=[a], eng="pool")
                    kb.tt(a[:], a[:], bb[:], ALU.add, R=[a, bb], W=[a])
                    if final:
                        kb.dma("sp", dst_d[(tt - 2) * 128:(tt - 1) * 128, :], a[:], R=[a], W=[kb.tok("out", tt)])
                    else:
                        kb.dma("sp", dst_d[tt * 128:(tt + 1) * 128, :], a[:], R=[a], W=[kb.tok("xres", tt)])


def full_stages():
    st = [lambda kb, cx: stage_mods(kb, cx)]
    for l in range(DEPTH):
        src = "xin" if l == 0 else "xres"
        wc = (l == 0)
        tt0 = 0 if l == 0 else 2
        st.append(lambda kb, cx, l=l, src=src: stage_a1(kb, cx, l, src))
        st.append(lambda kb, cx, l=l: stage_a1b(kb, cx, l))
        st.append(lambda kb, cx, l=l, wc=wc: stage_hyena(kb, cx, l, with_ctx=wc))
        st.append(lambda kb, cx, l=l: stage_lru(kb, cx, l))
        st.append(lambda kb, cx, l=l, wc=wc: stage_att(kb, cx, l, with_ctx=wc))
        st.append(lambda kb, cx, l=l, src=src, tt0=tt0: stage_b1(kb, cx, l, src, tt0))
        st.append(lambda kb, cx, l=l, tt0=tt0: stage_a1(kb, cx, l, "x1_d", jsh=3, jsc=4, dst="fT_d", tt0=tt0))
        st.append(lambda kb, cx, l=l, tt0=tt0: stage_moe(kb, cx, l, tt0, final=(l == DEPTH - 1)))
    return st


def build(stages, io_in=(), io_out=()):
    kb = KB(io_in, io_out)
    cx = Ctx()
    for s in stages:
        s(kb, cx)
    nc = kb.emit()
    return kb, nc


def kernel(**inputs):
    kb, nc = build(full_stages())
    hc = host_consts()
    B = inputs["x"].shape[0]
    in_maps = []
    for b in range(B):
        full = {
            "xin": np.ascontiguousarray(np.concatenate([inputs["ctx"][b], inputs["x"][b]], axis=0), dtype=np.float32),
            "crows": np.ascontiguousarray(np.stack([inputs["c"][b], inputs["c_ctx"]], axis=0), dtype=np.float32),
        }
        im = {}
        for name, (shape, dt) in kb.inputs.items():
            if name in full:
                a = full[name]
            elif name in hc:
                a = hc[name]
            else:
                a = np.ascontiguousarray(inputs[name])
            assert tuple(a.shape) == tuple(shape), (name, a.shape, shape)
            im[name] = a
        in_maps.append(im)
    res = run_bass_kernel_spmd(nc, in_maps, core_ids=list(range(B)))
    out = np.stack([np.asarray(res.results[b]["out"], dtype=np.float32) for b in range(B)], axis=0)
    return out
```

```python
import math
import contextlib
import numpy as np
import ml_dtypes
import concourse.bass as bass
import concourse.mybir as mybir
from concourse.bass_utils import run_bass_kernel_spmd

F32 = mybir.dt.float32
BF16 = mybir.dt.bfloat16
I32 = mybir.dt.int32
AF = mybir.ActivationFunctionType
ALU = mybir.AluOpType
AX = mybir.AxisListType

D = 2048
NCTX = 256
NLAT = 4096
NTOK = NCTX + NLAT
NTT = NTOK // 128
DEPTH = 2
D_IN = 5632
LN_EPS = 1e-5
ALPHA = (2 * DEPTH) ** 0.25
NEXP = 64
DEXP = 512

SAME_SYNC = True
ARENA_WORDS = 40 * 1024


class Tok:
    __slots__ = ("w", "r")

    def __init__(self):
        self.w = None
        self.r = {}


class T(Tok):
    __slots__ = ("ap",)

    def __init__(self, ap):
        Tok.__init__(self)
        self.ap = ap

    def __getitem__(self, k):
        return self.ap[k]


class KB:
    ENGS = ("sp", "act", "dve", "pool", "pe")
    ATTR = {"sp": "sync", "act": "scalar", "dve": "vector", "pool": "gpsimd", "pe": "tensor"}
    NDMA = 6

    def __init__(self, io_in=(), io_out=()):
        self.nc = bass.Bass("TRN2", target_bir_lowering=False)
        self.st = contextlib.ExitStack()
        self.q = {e: [] for e in self.ENGS}
        self.waited = {e: {} for e in self.ENGS}
        self.cnt = {}
        self.sem = {}
        self.rr = {e: 0 for e in self.ENGS}
        self.cur = {}
        self.gen = {}
        for e in self.ENGS:
            self._mksem("s_" + e)
            self.cur["s_" + e] = "s_" + e
            self.gen["s_" + e] = 0
            if e in ("sp", "pool"):
                for i in range(self.NDMA):
                    self._mksem(f"d_{e}_{i}")
                    self.cur[f"d_{e}_{i}"] = f"d_{e}_{i}"
                    self.gen[f"d_{e}_{i}"] = 0
        self.arena = self.st.enter_context(self.nc.sbuf_tensor("arena", [128, ARENA_WORDS], F32))
        self.top = 0
        self.psum = self.st.enter_context(self.nc.psum_tensor("psum", [128, 8, 512], F32))
        self.bank = [T(self.psum[:, i, :]) for i in range(8)]
        self.io_in = set(io_in)
        self.io_out = set(io_out)
        self.inputs = {}
        self.outputs = {}
        self.drams = {}
        self.dtok = {}
        self.eps = self.tile((1,))
        self.memset(self.eps[:], LN_EPS, W=[self.eps])

    def _mksem(self, name):
        self.sem[name] = self.st.enter_context(self.nc.semaphore(name))
        self.cnt[name] = 0

    def tile(self, free, dtype=F32, parts=128):
        n = int(np.prod(free))
        words = n if dtype in (F32, I32) else (n + 1) // 2
        words = (words + 15) // 16 * 16
        off = self.top
        self.top += words
        self.maxtop = max(getattr(self, 'maxtop', 0), self.top)
        assert self.top <= ARENA_WORDS, f"SBUF arena overflow {self.top}"
        ap = self.arena[:, off:off + words]
        if dtype != F32:
            ap = ap.bitcast(dtype)
        ap = ap[:, 0:n]
        if len(free) == 2:
            ap = ap.rearrange("p (a b) -> p a b", b=free[1])
        elif len(free) == 3:
            ap = ap.rearrange("p (a b c) -> p a b c", b=free[1], c=free[2])
        return T(ap)

    @contextlib.contextmanager
    def scope(self):
        mark = self.top
        yield
        self.barrier()
        self.top = mark

    def dram(self, name, shape, dtype):
        if name in self.drams:
            return self.drams[name]
        if name in self.io_in:
            t = self.nc.dram_tensor(name, list(shape), dtype, kind="ExternalInput")
            self.inputs[name] = (tuple(shape), dtype)
        elif name in self.io_out:
            t = self.nc.dram_tensor(name, list(shape), dtype, kind="ExternalOutput")
            self.outputs[name] = (tuple(shape), dtype)
        else:
            t = self.nc.dram_tensor(name, list(shape), dtype)
        self.drams[name] = t
        return t

    def inp(self, name, shape, dtype=F32):
        if name not in self.drams:
            self.io_in.add(name)
        return self.dram(name, shape, dtype)

    def tok(self, *key):
        t = self.dtok.get(key)
        if t is None:
            t = self.dtok[key] = Tok()
        return t

    def _wait(self, eng, evs):
        own = self.cur["s_" + eng]
        for sem, val in evs:
            if sem == own and (eng == "pe" or not SAME_SYNC):
                continue
            if self.waited[eng].get(sem, 0) >= val:
                continue
            self.waited[eng][sem] = val
            self.q[eng].append(("w", sem, val))

    def _deps(self, R, W):
        evs = []
        for t in R:
            if t.w is not None:
                evs.append(t.w)
        for t in W:
            if t.w is not None:
                evs.append(t.w)
            evs.extend(t.r.items())
        return evs

    def _mark(self, ev, R, W):
        sem, v = ev
        for t in R:
            if t.r.get(sem, 0) < v:
                t.r[sem] = v
        for t in W:
            t.w = ev
            t.r = {}

    def op(self, eng, fn, R=(), W=()):
        self._wait(eng, self._deps(R, W))
        sem = self.cur["s_" + eng]
        self.cnt[sem] += 1
        v = self.cnt[sem]
        self.q[eng].append(("o", fn, sem))
        self._mark((sem, v), R, W)

    def dma(self, eng, out, in_, R=(), W=(), **kw):
        i = self.rr[eng]
        self.rr[eng] = (i + 1) % self.NDMA
        sem = self.cur[f"d_{eng}_{i}"]
        evs = self._deps(R, W)
        if self.cnt[sem] > 0:
            evs.append((sem, self.cnt[sem]))
        self._wait(eng, evs)
        self.cnt[sem] += 16
        v = self.cnt[sem]
        self.q[eng].append(("d", out, in_, kw, sem))
        self._mark((sem, v), R, W)

    def barrier(self):
        evs = [(s, c) for s, c in self.cnt.items() if c > 0]
        for e in self.ENGS:
            self._wait(e, evs)
        for key in list(self.cur.keys()):
            name = self.cur[key]
            if self.cnt[name] > 12000:
                self.gen[key] += 1
                nn = f"{key}_g{self.gen[key]}"
                self._mksem(nn)
                self.cur[key] = nn

    def checkpoint(self):
        if max(self.cnt[self.cur[k]] for k in self.cur) > 12000:
            self.barrier()

    def emit(self):
        self.barrier()
        nc = self.nc
        with nc.Block() as block:
            for eng in self.ENGS:
                items = self.q[eng]
                sems = self.sem

                def body(e, items=items):
                    for it in items:
                        if it[0] == "w":
                            e.wait_ge(sems[it[1]], it[2])
                        elif it[0] == "o":
                            it[1](e).then_inc(sems[it[2]], 1)
                        else:
                            e.dma_start(out=it[1], in_=it[2], **it[3]).then_inc(sems[it[4]], 16)

                getattr(block, self.ATTR[eng])(body)
        return nc

    def mm(self, out, lhsT, rhs, start, stop, R, W):
        self.op("pe", lambda e: e.matmul(out, lhsT, rhs, start=start, stop=stop), R, W)

    def tr(self, out, in_, ident, R, W):
        self.op("pe", lambda e: e.transpose(out, in_, ident), R, W)

    def act(self, out, in_, func, R, W, bias=None, scale=None, accum_out=None):
        kw = {}
        if bias is not None:
            kw["bias"] = bias
        if scale is not None:
            kw["scale"] = scale
        if accum_out is not None:
            kw["accum_out"] = accum_out
        self.op("act", lambda e: e.activation(out=out, in_=in_, func=func, **kw), R, W)

    def tt(self, out, in0, in1, op, R, W, eng="dve"):
        self.op(eng, lambda e: e.tensor_tensor(out=out, in0=in0, in1=in1, op=op), R, W)

    def ts(self, out, in0, s1, s2, op0, op1, R, W, eng="dve", accum_out=None):
        if op1 is None:
            self.op(eng, lambda e: e.tensor_scalar(out=out, in0=in0, scalar1=s1, scalar2=None, op0=op0), R, W)
        elif accum_out is not None:
            self.op(eng, lambda e: e.tensor_scalar(out=out, in0=in0, scalar1=s1, scalar2=s2, op0=op0, op1=op1,
                                                   accum_out=accum_out), R, W)
        else:
            self.op(eng, lambda e: e.tensor_scalar(out=out, in0=in0, scalar1=s1, scalar2=s2, op0=op0, op1=op1), R, W)

    def stt(self, out, in0, scalar, in1, op0, op1, R, W):
        self.op("dve", lambda e: e.scalar_tensor_tensor(out=out, in0=in0, scalar=scalar, in1=in1, op0=op0, op1=op1), R, W)

    def copy(self, out, in_, R, W, eng="dve"):
        if eng == "act":
            self.op("act", lambda e: e.copy(out=out, in_=in_), R, W)
        else:
            self.op(eng, lambda e: e.tensor_copy(out=out, in_=in_), R, W)

    def memset(self, ap, val, W, eng="dve"):
        self.op(eng, lambda e: e.memset(ap, val), (), W)


def host_consts():
    c = {}
    c["ident_f"] = np.eye(128, dtype=np.float32)
    c["ident_b"] = np.eye(128, dtype=np.float32).astype(ml_dtypes.bfloat16)
    return c


CONST_SHAPES = {
    "ident_f": ((128, 128), F32),
    "ident_b": ((128, 128), BF16),
}


class Ctx:
    pass


def load_const(kb, cx, name):
    shape, dt = CONST_SHAPES[name]
    d = kb.inp(name, shape, dt)
    t = kb.tile(shape[1:], dt)
    kb.dma("sp", t[0:shape[0]], d.ap(), R=[kb.tok(name)], W=[t])
    return t


def stage_mods(kb, cx, layers=(0, 1)):
    crows = kb.inp("crows", (2, D))
    ada_w = kb.inp("ada_w", (DEPTH, D, 6 * D))
    ada_b = kb.inp("ada_b", (DEPTH, 6 * D))
    mod_d = kb.dram("mod_d", (DEPTH, 2, 6 * D), F32)
    with kb.scope():
        scT = kb.tile((2, 16))
        for r in range(2):
            kb.dma("sp", scT[:, r, :], crows[r, :].rearrange("(kc p) -> p kc", p=128), R=[kb.tok("crows")], W=[scT],
                   allow_slow_non_contiguous=True)
        kb.act(scT[:], scT[:], AF.Silu, R=[scT], W=[scT])
        wt = [kb.tile((2048,)) for _ in range(3)]
        bt = kb.tile((2048,))
        ot = kb.tile((2048,))
        n = 0
        for l in layers:
            for j in range(6):
                for kc in range(16):
                    w = wt[n % 3]
                    n += 1
                    kb.dma("sp", w[:], ada_w[l, kc * 128:(kc + 1) * 128, j * 2048:(j + 1) * 2048],
                           R=[kb.tok("ada_w")], W=[w])
                    for ct in range(4):
                        kb.mm(kb.bank[ct][0:2, :], scT[:, :, kc], w[:, ct * 512:(ct + 1) * 512],
                              start=(kc == 0), stop=(kc == 15), R=[scT, w], W=[kb.bank[ct]])
                kb.dma("sp", bt[0:2, :], ada_b[l, j * 2048:(j + 1) * 2048].partition_broadcast(2),
                       R=[kb.tok("ada_b")], W=[bt])
                for ct in range(4):
                    kb.tt(ot[0:2, ct * 512:(ct + 1) * 512], kb.bank[ct][0:2, :], bt[0:2, ct * 512:(ct + 1) * 512],
                          ALU.add, R=[kb.bank[ct], bt], W=[ot])
                if j in (1, 4):
                    kb.ts(ot[0:2, :], ot[0:2, :], 1.0, None, ALU.add, None, R=[ot], W=[ot])
                kb.dma("sp", mod_d[l, :, j * 2048:(j + 1) * 2048], ot[0:2, :], R=[ot], W=[kb.tok("mod_d", l, j)])


def load_mod(kb, l, j, r, t):
    mod_d = kb.dram("mod_d", (DEPTH, 2, 6 * D), F32)
    kb.dma("sp", t[:], mod_d[l, r, j * 2048:(j + 1) * 2048].partition_broadcast(128),
           R=[kb.tok("mod_d", l, j)], W=[t])


def layer_norm_tile(kb, xt, rows, st, mv, rstd, W_extra=()):
    for c4 in range(4):
        kb.op("dve", lambda e, c4=c4: e.bn_stats(out=st[0:rows, c4, :], in_=xt[0:rows, c4 * 512:(c4 + 1) * 512]),
              R=[xt], W=[st])
    kb.op("dve", lambda e: e.bn_aggr(out=mv[0:rows, :], in_=st[0:rows, :, :].rearrange("p a b -> p (a b)")), R=[st], W=[mv])
    kb.act(rstd[0:rows, :], mv[0:rows, 1:2], AF.Sqrt, R=[mv, kb.eps], W=[rstd], bias=cx_eps(kb)[0:rows, :], scale=1.0)
    kb.op("dve", lambda e: e.reciprocal(out=rstd[0:rows, :], in_=rstd[0:rows, :]), R=[rstd], W=[rstd])


def cx_eps(kb):
    return kb.eps


def stage_a1(kb, cx, l, src_name, jsh=0, jsc=1, dst="hT_d", tt0=0):
    x_d = kb.inp(src_name, (NTOK, D)) if src_name == "xin" else kb.dram(src_name, (NTOK, D), F32)
    hT_d = kb.dram(dst, (128, 16, NTOK), BF16)
    with kb.scope():
        ident = load_const(kb, cx, "ident_b")
        sh = [kb.tile((2048,)) for _ in range(2)]
        sc = [kb.tile((2048,)) for _ in range(2)]
        for r in range(2):
            load_mod(kb, l, jsh, r, sh[r])
            load_mod(kb, l, jsc, r, sc[r])
        xt = [kb.tile((2048,)) for _ in range(2)]
        st = kb.tile((4, 6))
        mv = kb.tile((2,))
        rstd = kb.tile((1,))
        xn = kb.tile((2048,))
        hb = [kb.tile((2048,), BF16) for _ in range(2)]
        hT = [kb.tile((16, 128), BF16) for _ in range(2)]
        for tt in range(tt0, NTT):
            r = 1 if tt < 2 else 0
            x = xt[tt % 2]
            kb.dma("sp", x[:], x_d[tt * 128:(tt + 1) * 128, :], R=[kb.tok(src_name, tt)], W=[x])
            layer_norm_tile(kb, x, 128, st, mv, rstd)
            kb.ts(xn[:], x[:], mv[:, 0:1], rstd[:, 0:1], ALU.subtract, ALU.mult, R=[x, mv, rstd], W=[xn])
            kb.tt(xn[:], xn[:], sc[r][:], ALU.mult, R=[xn, sc[r]], W=[xn], eng="pool")
            h = hb[tt % 2]
            kb.tt(h[:], xn[:], sh[r][:], ALU.add, R=[xn, sh[r]], W=[h])
            o = hT[tt % 2]
            for half in range(2):
                bk = kb.bank[(tt % 2) * 2 + half]
                pv = bk.ap.bitcast(BF16).rearrange("p (a b) -> p a b", b=128)
                for k8 in range(8):
                    kc = half * 8 + k8
                    kb.tr(pv[:, k8, :], h[:, kc * 128:(kc + 1) * 128], ident[:], R=[h, ident], W=[bk])
                kb.copy(o[:, half * 8:(half + 1) * 8, :], pv[:, :, :], R=[bk], W=[o], eng=("act" if half else "dve"))
            kb.dma("sp", hT_d[:, :, tt * 128:(tt + 1) * 128], o[:], R=[o], W=[kb.tok(dst, tt // 4)])


def stage_a1b(kb, cx, l):
    w_in = kb.inp("w_in", (DEPTH, D, D_IN))
    hT_d = kb.dram("hT_d", (128, 16, NTOK), BF16)
    uT_d = kb.dram("uT_d", (36, 128, NTOK), F32)
    v_d = kb.dram("v_d", (NTOK, 1024), BF16)
    tiles = [(0, 256)] + [(256 + i * 512, 512) for i in range(8)]
    with kb.scope():
        wt = [kb.tile((16, 512), BF16) for _ in range(2)]
        ht = [kb.tile((16, 512), BF16) for _ in range(2)]
        us = [kb.tile((4, 512)) for _ in range(2)]
        vs = [kb.tile((512,), BF16) for _ in range(2)]
        n = 0
        m = 0
        for cg in range(11):
            w = wt[cg % 2]
            for q4 in range(4):
                kb.dma("pool", w[:, q4 * 4:(q4 + 1) * 4, :],
                       w_in[l, q4 * 512:(q4 + 1) * 512, cg * 512:(cg + 1) * 512].rearrange("(kc p) c -> p kc c", p=128),
                       R=[kb.tok("w_in")], W=[w])
            for (t0, nt) in tiles:
                h = ht[n % 2]
                kb.dma("sp", h[:, :, 0:nt], hT_d[:, :, t0:t0 + nt],
                       R=[kb.tok("hT_d", i) for i in range(t0 // 512, (t0 + nt - 1) // 512 + 1)], W=[h])
                if cg < 9:
                    u = us[n % 2]
                    for j in range(4):
                        bk = kb.bank[(n % 2) * 4 + j]
                        for kc in range(16):
                            kb.mm(bk[:, 0:nt], w[:, kc, j * 128:(j + 1) * 128], h[:, kc, 0:nt],
                                  start=(kc == 0), stop=(kc == 15), R=[w, h], W=[bk])
                        kb.copy(u[:, j, 0:nt], bk[:, 0:nt], R=[bk], W=[u], eng=("act" if j % 2 else "dve"))
                    kb.dma("sp", uT_d[cg * 4:(cg + 1) * 4, :, t0:t0 + nt].rearrange("g p t -> p g t"), u[:, :, 0:nt],
                           R=[u], W=[kb.tok("uT_d", cg * 4 + j) for j in range(4)])
                else:
                    for s in range(nt // 128):
                        bk = kb.bank[m % 4]
                        v = vs[m % 2]
                        m += 1
                        for kc in range(16):
                            kb.mm(bk[:, :], h[:, kc, s * 128:(s + 1) * 128], w[:, kc, :],
                                  start=(kc == 0), stop=(kc == 15), R=[w, h], W=[bk])
                        kb.copy(v[:], bk[:, :], R=[bk], W=[v], eng=("act" if m % 2 else "dve"))
                        kb.dma("sp", v_d[t0 + s * 128:t0 + (s + 1) * 128, (cg - 9) * 512:(cg - 8) * 512], v[:],
                               R=[v], W=[kb.tok("v_d")])
                n += 1


def hy_feats(n):
    pos = np.arange(n, dtype=np.float32)
    t = (pos / np.float32(max(n - 1, 1)))[:, None]
    bands = np.linspace(1e-4, 7, 8, dtype=np.float32)
    ang = np.float32(2.0 * math.pi / n) * pos[:, None] * bands
    feats = np.concatenate([t, np.cos(ang), -np.sin(ang)], axis=-1).astype(np.float32)
    return np.ascontiguousarray(feats.T)


def hy_env(n):
    pos = np.arange(n, dtype=np.float32)
    t = (pos / np.float32(max(n - 1, 1)))[:, None]
    mx = math.log(1e-2) / 0.3
    mn = math.log(1e-2) / 1.5
    decay = np.abs(np.linspace(mn, mx, 512, dtype=np.float32))
    env = np.exp(-t * decay).astype(np.float32)
    return np.ascontiguousarray(env.T)


def rope_tables():
    n = NLAT
    rows = n // 64
    row = np.broadcast_to(np.arange(rows, dtype=np.float32)[:, None], (rows, 64)).reshape(-1)
    col = np.broadcast_to(np.arange(64, dtype=np.float32)[None, :], (rows, 64)).reshape(-1)
    inv = (10000.0 ** (-np.arange(0, 64, 2, dtype=np.float32) / 64)).astype(np.float32)
    ang = np.stack([row[:, None] * inv, col[:, None] * inv], axis=1)
    cos = np.cos(ang).astype(np.float32)
    sin = np.sin(ang).astype(np.float32)
    cosT = np.zeros((128, n), np.float32)
    sinT = np.zeros((128, n), np.float32)
    P = np.zeros((128, 128), np.float32)
    for ax in range(2):
        for half in range(2):
            for f in range(32):
                d = ax * 64 + half * 32 + f
                cosT[d] = cos[:, ax, f]
                sinT[d] = sin[:, ax, f]
                if half == 0:
                    P[d + 32, d] = -1.0
                else:
                    P[d - 32, d] = 1.0
    return cosT, sinT, P


def host_consts():
    c = {}
    c["ident_f"] = np.eye(128, dtype=np.float32)
    c["ident_b"] = np.eye(128, dtype=np.float32).astype(ml_dtypes.bfloat16)
    c["ones_f"] = np.ones((128, 128), np.float32)
    c["featsT_4096"] = hy_feats(4096)
    c["featsT_256"] = hy_feats(256)
    c["env_4096"] = hy_env(4096)
    c["env_256"] = hy_env(256)
    cosT, sinT, P = rope_tables()
    c["rope_cos"] = cosT
    c["rope_sin"] = sinT
    c["rope_P"] = P
    for n in (256, 4096):
        C_, S_ = dft_tables(n)
        c[f"dftC_{n}"] = C_
        c[f"dftS_{n}"] = S_
    alt = np.ones((128, NLAT), np.float32)
    alt[:, 1::2] = -1.0
    c["alt_sign"] = alt.astype(ml_dtypes.bfloat16)
    return c


CONST_SHAPES.update({
    "ones_f": ((128, 128), F32),
    "rope_P": ((128, 128), F32),
})


def conv_seg(kb, out, src, cw, cb, g, taps, left, off, n, R, W):
    o = out[:, off:off + n]
    kb.ts(o, src[:, off:off + n], cw[:, left, g:g + 1], cb[:, g:g + 1], ALU.mult, ALU.add, R=R, W=W)
    for k in range(taps):
        sft = k - left
        if sft == 0:
            continue
        if sft < 0:
            a = -sft
            kb.stt(out[:, off + a:off + n], src[:, off:off + n - a], cw[:, k, g:g + 1], out[:, off + a:off + n],
                   ALU.mult, ALU.add, R=R + W, W=W)
        else:
            kb.stt(out[:, off:off + n - sft], src[:, off + sft:off + n], cw[:, k, g:g + 1], out[:, off:off + n - sft],
                   ALU.mult, ALU.add, R=R + W, W=W)


def dft_tables(n):
    t = np.arange(n, dtype=np.int64)
    m = (t[:, None] * t[None, :]) % (2 * n)
    ang = np.pi * m.astype(np.float64) / n
    return np.cos(ang).astype(ml_dtypes.bfloat16), np.sin(ang).astype(ml_dtypes.bfloat16)


HY_STOP = [99]
HY_SEGS = [None]


def stage_hyena(kb, cx, l, groups=(0, 1, 2, 3), with_ctx=True):
    uT_d = kb.dram("uT_d", (36, 128, NTOK), F32)
    yT_d = kb.dram("yT_d", (16, 128, NTOK), BF16)
    cwd = kb.inp("hy_conv_w", (DEPTH, 3, 1536))
    cbd = kb.inp("hy_conv_b", (DEPTH, 1536))
    w1d = kb.inp("hy_w1", (DEPTH, 17, 64))
    b1d = kb.inp("hy_b1", (DEPTH, 64))
    w2d = kb.inp("hy_w2", (DEPTH, 64, 64))
    b2d = kb.inp("hy_b2", (DEPTH, 64))
    w3d = kb.inp("hy_w3", (DEPTH, 64, 2048))
    frd = kb.inp("hy_freq", (DEPTH, 64))
    skd = kb.inp("hy_skip", (DEPTH, 2, 512))
    altd = kb.inp("alt_sign", (128, NLAT), BF16)
    segs = ([(0, NCTX)] if with_ctx else []) + [(NCTX, NLAT)]
    if HY_SEGS[0] is not None:
        segs = HY_SEGS[0]
    with kb.scope():
        ident = load_const(kb, cx, "ident_b")
        cw = kb.tile((3, 12))
        cb = kb.tile((12,))
        sk = kb.tile((2, 4))
        for k in range(3):
            kb.dma("sp", cw[:, k, :], cwd[l, k, :].rearrange("(g p) -> p g", p=128), R=[kb.tok("hyp")], W=[cw],
                   allow_slow_non_contiguous=True)
        kb.dma("sp", cb[:], cbd[l, :].rearrange("(g p) -> p g", p=128), R=[kb.tok("hyp")], W=[cb],
               allow_slow_non_contiguous=True)
        for o in range(2):
            kb.dma("sp", sk[:, o, :], skd[l, o, :].rearrange("(g p) -> p g", p=128), R=[kb.tok("hyp")], W=[sk],
                   allow_slow_non_contiguous=True)
        w1 = kb.tile((64,))
        w2 = kb.tile((64,))
        w3 = kb.tile((2048,), BF16)
        fr = kb.tile((1,))
        fb1 = kb.tile((1,))
        fb2 = kb.tile((1,))
        kb.dma("sp", w1[0:17, :], w1d[l], R=[kb.tok("hyp")], W=[w1])
        kb.dma("sp", w2[0:64, :], w2d[l], R=[kb.tok("hyp")], W=[w2])
        kb.dma("pool", w3[0:64, :], w3d[l], R=[kb.tok("hyp")], W=[w3])
        kb.dma("sp", fr[0:64, :], frd[l, :].rearrange("(p o) -> p o", o=1), R=[kb.tok("hyp")], W=[fr])
        kb.dma("sp", fb1[0:64, :], b1d[l, :].rearrange("(p o) -> p o", o=1), R=[kb.tok("hyp")], W=[fb1])
        kb.dma("sp", fb2[0:64, :], b2d[l, :].rearrange("(p o) -> p o", o=1), R=[kb.tok("hyp")], W=[fb2])
        kb.tt(fb1[0:64, :], fb1[0:64, :], fr[0:64, :], ALU.mult, R=[fb1, fr], W=[fb1])
        kb.tt(fb2[0:64, :], fb2[0:64, :], fr[0:64, :], ALU.mult, R=[fb2, fr], W=[fb2])
        for (off, n) in segs:
            nk = n // 128
            fw = min(256, n)
            nft = n // fw
            Cd = kb.inp(f"dftC_{n}", (n, n), BF16)
            Sd = kb.inp(f"dftS_{n}", (n, n), BF16)
            with kb.scope():
                hid2 = kb.tile((n,), BF16)
                negpi = kb.tile((1,))
                kb.memset(negpi[:], -math.pi, W=[negpi])
                with kb.scope():
                    ft = kb.tile((n,))
                    fd = kb.inp(f"featsT_{n}", (17, n))
                    kb.dma("sp", ft[0:17, :], fd.ap(), R=[kb.tok("hyp")], W=[ft])
                    hid1 = kb.tile((n,))
                    hid2f = kb.tile((n,))
                    rri = kb.tile((512,), I32)
                    rrf = kb.tile((512,))
                    tl = min(512, n)
                    for (src, wt, kk, fb, dst) in ((ft, w1, 17, fb1, hid1), (hid1, w2, 64, fb2, hid2f)):
                        for i in range(n // tl):
                            bk = kb.bank[i % 4]
                            kb.mm(bk[0:64, 0:tl], wt[0:kk, 0:64], src[0:kk, i * tl:(i + 1) * tl], True, True, R=[wt, src], W=[bk])
                            d = dst[0:64, i * tl:(i + 1) * tl]
                            kb.ts(d, bk[0:64, 0:tl], fr[0:64, 0:1], fb[0:64, 0:1], ALU.mult, ALU.add, R=[bk, fr, fb], W=[dst])
                            yi = rri[0:64, 0:tl]
                            yf = rrf[0:64, 0:tl]
                            kb.ts(d, d, 1.0 / (2.0 * math.pi), 16.5, ALU.mult, ALU.add, R=[dst], W=[dst])
                            kb.copy(yi, d, R=[dst], W=[rri])
                            kb.copy(yf, yi, R=[rri], W=[rrf])
                            kb.tt(d, d, yf, ALU.subtract, R=[dst, rrf], W=[dst])
                            kb.ts(yf, d, 0.0, None, ALU.is_lt, None, R=[dst], W=[rrf])
                            kb.tt(d, d, yf, ALU.add, R=[dst, rrf], W=[dst])
                            kb.act(d, d, AF.Sin, R=[dst, negpi], W=[dst], bias=negpi[0:64, :], scale=2.0 * math.pi)
                    kb.copy(hid2[0:64, :], hid2f[0:64, :], R=[hid2f], W=[hid2])
                envd = kb.inp(f"env_{n}", (512, n))
                raw = kb.tile((n,))
                z = kb.tile((n,))
                fwd = kb.tile((n,))
                bwd = kb.tile((n,))
                bh = kb.tile((n,))
                b16 = kb.tile((n,), BF16)
                aT = kb.tile((nk, 128), BF16)
                sT = kb.tile((nk, 128), BF16)
                zT = kb.tile((nk, 128), BF16)
                tab = kb.tile((nk, fw), BF16)
                alt = kb.tile((n,), BF16)
                scl = kb.tile((8,))
                t1 = kb.tile((fw,))
                t2 = kb.tile((fw,))
                yb = b16
                kb.dma("sp", alt[:], altd[:, 0:n], R=[kb.tok("hyp")], W=[alt])

                def to_tm(src16, dstT, tok_=None):
                    tok_ = src16 if tok_ is None else tok_
                    for k8 in range(0, nk, 8):
                        m8 = min(8, nk - k8)
                        bk = kb.bank[7]
                        pv = bk.ap.bitcast(BF16).rearrange("p (a b) -> p a b", b=128)
                        for j in range(m8):
                            kb.tr(pv[:, j, :], src16[:, (k8 + j) * 128:(k8 + j + 1) * 128], ident[:], R=[tok_, ident], W=[bk])
                        kb.copy(dstT[:, k8:k8 + m8, :], pv[:, 0:m8, :], R=[bk], W=[dstT], eng="act")

                def load_tab(src, fi):
                    for k8 in range(0, nk, 8):
                        m8 = min(8, nk - k8)
                        kb.dma("sp", tab[:, k8:k8 + m8, :],
                               src[k8 * 128:(k8 + m8) * 128, fi * fw:(fi + 1) * fw].rearrange("(kc p) f -> p kc f", p=128),
                               R=[kb.tok("hyp")], W=[tab])

                rawb = raw.ap.bitcast(BF16)
                p16 = rawb[:, 0:n]
                q16 = rawb[:, n:2 * n]

                for g in groups:
                    kb.dma("sp", raw[:], uT_d[8 + g, :, off:off + n], R=[kb.tok("uT_d", 8 + g)], W=[raw])
                    conv_seg(kb, z, raw, cw, cb, 8 + g, 3, 1, 0, n, R=[raw, cw, cb], W=[z])
                    for o in range(2):
                        kb.checkpoint()
                        env = bh
                        kb.dma("sp", env[:], envd[g * 128:(g + 1) * 128, :], R=[kb.tok("hyp")], W=[env])
                        for s_, dst in ((0, fwd), (1, bwd)):
                            c0 = s_ * 1024 + o * 512 + g * 128
                            for i in range(n // tl):
                                bk = kb.bank[4 + i % 3]
                                kb.mm(bk[:, 0:tl], w3[0:64, c0:c0 + 128], hid2[0:64, i * tl:(i + 1) * tl], True, True,
                                      R=[w3, hid2], W=[bk])
                                kb.tt(dst[:, i * tl:(i + 1) * tl], bk[:, 0:tl], env[:, i * tl:(i + 1) * tl], ALU.mult,
                                      R=[bk, env], W=[dst])
                        if HY_STOP[0] <= 0:
                            continue
                        kb.memset(bwd[:, 0:1], 0.0, W=[bwd])
                        kb.act(raw[:], fwd[:], AF.Abs, R=[fwd], W=[raw])
                        kb.op("dve", lambda e, scl=scl, raw=raw: e.reduce_sum(out=scl[:, 0:1], in_=raw[:], axis=AX.X), R=[raw], W=[scl])
                        kb.act(raw[:], bwd[:], AF.Abs, R=[bwd], W=[raw])
                        kb.op("dve", lambda e, scl=scl, raw=raw: e.reduce_sum(out=scl[:, 1:2], in_=raw[:], axis=AX.X), R=[raw], W=[scl])
                        kb.tt(scl[:, 2:3], scl[:, 0:1], scl[:, 1:2], ALU.add, R=[scl], W=[scl])
                        kb.ts(scl[:, 2:3], scl[:, 2:3], float(n), None, ALU.mult, None, R=[scl], W=[scl])
                        kb.op("dve", lambda e, scl=scl: e.reciprocal(out=scl[:, 3:4], in_=scl[:, 2:3]), R=[scl], W=[scl])
                        kb.tt(raw[:], fwd[:], bwd[:], ALU.add, R=[fwd, bwd], W=[raw])
                        kb.tt(bwd[:], fwd[:], bwd[:], ALU.subtract, R=[fwd, bwd], W=[bwd], eng="pool")
                        kb.tt(fwd[:], raw[:], alt[:], ALU.mult, R=[raw, alt], W=[fwd])
                        kb.op("dve", lambda e, scl=scl, fwd=fwd: e.reduce_sum(out=scl[:, 4:5], in_=fwd[:], axis=AX.X), R=[fwd], W=[scl])
                        kb.tt(fwd[:], z[:], alt[:], ALU.mult, R=[z, alt], W=[fwd])
                        kb.op("dve", lambda e, scl=scl, fwd=fwd: e.reduce_sum(out=scl[:, 5:6], in_=fwd[:], axis=AX.X), R=[fwd], W=[scl])
                        kb.tt(scl[:, 6:7], scl[:, 4:5], scl[:, 5:6], ALU.mult, R=[scl], W=[scl])
                        kb.ts(scl[:, 6:7], scl[:, 6:7], 0.5, None, ALU.mult, None, R=[scl], W=[scl])
                        if HY_STOP[0] <= 1:
                            continue
                        kb.copy(b16[:], raw[:], R=[raw], W=[b16], eng="act")
                        to_tm(b16, aT)
                        kb.copy(b16[:], bwd[:], R=[bwd], W=[b16], eng="act")
                        to_tm(b16, sT)
                        kb.copy(b16[:], z[:], R=[z], W=[b16], eng="act")
                        to_tm(b16, zT)
                        if HY_STOP[0] <= 2:
                            continue
                        kb.dma("sp", raw[:], uT_d[4 * o + g, :, off:off + n], R=[kb.tok("uT_d", 4 * o + g)], W=[raw])
                        gate = bwd
                        conv_seg(kb, gate, raw, cw, cb, 4 * o + g, 3, 1, 0, n, R=[raw, cw, cb], W=[gate])
                        if HY_STOP[0] <= 3:
                            continue
                        for fi in range(nft):
                            b0, b1_ = kb.bank[(fi % 2) * 2], kb.bank[(fi % 2) * 2 + 1]
                            load_tab(Cd, fi)
                            for kc in range(nk):
                                kb.mm(b0[:, 0:fw], aT[:, kc, :], tab[:, kc, :], (kc == 0), (kc == nk - 1), R=[aT, tab], W=[b0])
                            load_tab(Sd, fi)
                            for kc in range(nk):
                                kb.mm(b1_[:, 0:fw], sT[:, kc, :], tab[:, kc, :], (kc == 0), (kc == nk - 1), R=[sT, tab], W=[b1_])
                            kb.copy(fwd[:, fi * fw:(fi + 1) * fw], b0[:, 0:fw], R=[b0], W=[fwd])
                            kb.copy(bh[:, fi * fw:(fi + 1) * fw], b1_[:, 0:fw], R=[b1_], W=[bh], eng="act")
                        kb.ts(fwd[:, 0:1], fwd[:, 0:1], 0.5, None, ALU.mult, None, R=[fwd], W=[fwd])
                        if HY_STOP[0] <= 4:
                            continue
                        for fi in range(nft):
                            b0, b1_ = kb.bank[(fi % 2) * 2], kb.bank[(fi % 2) * 2 + 1]
                            load_tab(Cd, fi)
                            for kc in range(nk):
                                kb.mm(b0[:, 0:fw], zT[:, kc, :], tab[:, kc, :], (kc == 0), (kc == nk - 1), R=[zT, tab], W=[b0])
                            load_tab(Sd, fi)
                            for kc in range(nk):
                                kb.mm(b1_[:, 0:fw], zT[:, kc, :], tab[:, kc, :], (kc == 0), (kc == nk - 1), R=[zT, tab], W=[b1_])
                            fs_ = slice(fi * fw, (fi + 1) * fw)
                            kb.tt(t1[:], fwd[:, fs_], b0[:, 0:fw], ALU.mult, R=[fwd, b0], W=[t1])
                            kb.tt(t2[:], bh[:, fs_], b1_[:, 0:fw], ALU.mult, R=[bh, b1_], W=[t2])
                            kb.tt(p16[:, fs_], t1[:], t2[:], ALU.subtract, R=[t1, t2], W=[raw])
                            kb.tt(t1[:], fwd[:, fs_], b1_[:, 0:fw], ALU.mult, R=[fwd, b1_], W=[t1])
                            kb.tt(t2[:], bh[:, fs_], b0[:, 0:fw], ALU.mult, R=[bh, b0], W=[t2])
                            kb.tt(q16[:, fs_], t1[:], t2[:], ALU.add, R=[t1, t2], W=[raw])
                        to_tm(p16, aT, raw)
                        to_tm(q16, sT, raw)
                        if HY_STOP[0] <= 5:
                            continue
                        for ti in range(nft):
                            bk = kb.bank[4 + ti % 3]
                            load_tab(Cd, ti)
                            for kc in range(nk):
                                kb.mm(bk[:, 0:fw], aT[:, kc, :], tab[:, kc, :], (kc == 0), False, R=[aT, tab], W=[bk])
                            load_tab(Sd, ti)
                            for kc in range(nk):
                                kb.mm(bk[:, 0:fw], sT[:, kc, :], tab[:, kc, :], False, (kc == nk - 1), R=[sT, tab], W=[bk])
                            kb.stt(raw[:, ti * fw:(ti + 1) * fw], alt[:, ti * fw:(ti + 1) * fw], scl[:, 6:7], bk[:, 0:fw],
                                   ALU.mult, ALU.add, R=[alt, scl, bk], W=[raw])
                        kb.ts(raw[:], raw[:], scl[:, 3:4], None, ALU.mult, None, R=[raw, scl], W=[raw])
                        kb.stt(raw[:], z[:], sk[:, o, g:g + 1], raw[:], ALU.mult, ALU.add, R=[z, sk, raw], W=[raw])
                        kb.tt(z[:], raw[:], gate[:], ALU.mult, R=[raw, gate], W=[z])
                    kb.copy(yb[:], z[:], R=[z], W=[yb], eng="act")
                    kb.dma("sp", yT_d[g, :, off:off + n], yb[:], R=[yb], W=[kb.tok("yT_d", g)])


def stage_lru(kb, cx, l, groups=(0, 1, 2, 3)):
    uT_d = kb.dram("uT_d", (36, 128, NTOK), F32)
    yT_d = kb.dram("yT_d", (16, 128, NTOK), BF16)
    cwd = kb.inp("lru_conv_w", (DEPTH, 4, 512))
    cbd = kb.inp("lru_conv_b", (DEPTH, 512))
    wad = kb.inp("lru_wa", (DEPTH, 2, 8, 64, 64))
    bad = kb.inp("lru_ba", (DEPTH, 2, 512))
    wxd = kb.inp("lru_wx", (DEPTH, 2, 8, 64, 64))
    bxd = kb.inp("lru_bx", (DEPTH, 2, 512))
    lmd = kb.inp("lru_lambda", (DEPTH, 2, 512))
    N = NTOK
    with kb.scope():
        cw = kb.tile((4, 4))
        cb = kb.tile((4,))
        for k in range(4):
            kb.dma("sp", cw[:, k, :], cwd[l, k, :].rearrange("(g p) -> p g", p=128), R=[kb.tok("lrp")], W=[cw],
                   allow_slow_non_contiguous=True)
        kb.dma("sp", cb[:], cbd[l, :].rearrange("(g p) -> p g", p=128), R=[kb.tok("lrp")], W=[cb], allow_slow_non_contiguous=True)
        bia = kb.tile((3, 2, 4))
        for i, src in enumerate((bad, bxd, lmd)):
            for d in range(2):
                kb.dma("sp", bia[:, i, d, :], src[l, d, :].rearrange("(g p) -> p g", p=128), R=[kb.tok("lrp")], W=[bia],
                       allow_slow_non_contiguous=True)
        sp_ = kb.tile((2, 4))
        n8 = kb.tile((2, 4))
        n16 = kb.tile((2, 4))
        kb.act(sp_[:], bia[:, 2, :, :], AF.Exp, R=[bia], W=[sp_], scale=-1.0)
        kb.act(sp_[:], sp_[:], AF.Ln, R=[sp_], W=[sp_], bias=1.0, scale=1.0)
        kb.ts(n8[:], sp_[:], -8.0, None, ALU.mult, None, R=[sp_], W=[n8])
        kb.ts(n16[:], sp_[:], -16.0, None, ALU.mult, None, R=[sp_], W=[n16])
        wbd = kb.tile((2, 2, 128))
        raw = kb.tile((N,))
        xr = kb.tile((N,))
        gt = kb.tile((N,))
        rr = kb.tile((N,))
        ii = kb.tile((N,))
        aa = kb.tile((N,))
        hh = kb.tile((N,))
        h2 = kb.tile((N,))
        yb = kb.tile((N,), BF16)
        tiles = [(0, 256)] + [(256 + i * 512, 512) for i in range(8)]
        for g in groups:
            kb.memset(wbd[:], 0.0, W=[wbd])
            for i, src in enumerate((wad, wxd)):
                for d in range(2):
                    for hh_ in range(2):
                        kb.dma("sp", wbd[hh_ * 64:(hh_ + 1) * 64, i, d, hh_ * 64:(hh_ + 1) * 64], src[l, d, 2 * g + hh_],
                               R=[kb.tok("lrp")], W=[wbd])
            kb.dma("sp", raw[:], uT_d[16 + g, :, :], R=[kb.tok("uT_d", 16 + g)], W=[raw])
            kb.dma("sp", gt[:], uT_d[12 + g, :, :], R=[kb.tok("uT_d", 12 + g)], W=[gt])
            for (off, n) in ((0, NCTX), (NCTX, NLAT)):
                conv_seg(kb, xr, raw, cw, cb, g, 4, 2, off, n, R=[raw, cw, cb], W=[xr])
            for d in range(2):
                for i, dst in ((0, rr), (1, ii)):
                    for ti, (t0, nt) in enumerate(tiles):
                        bk = kb.bank[ti % 4 + 4 * i]
                        kb.mm(bk[:, 0:nt], wbd[:, i, d, :], xr[:, t0:t0 + nt], True, True, R=[wbd, xr], W=[bk])
                        kb.act(dst[:, t0:t0 + nt], bk[:, 0:nt], AF.Sigmoid, R=[bk, bia], W=[dst], bias=bia[:, i, d, g:g + 1], scale=1.0)
                kb.act(aa[:], rr[:], AF.Exp, R=[rr, n8], W=[aa], scale=n8[:, d, g:g + 1])
                kb.act(rr[:], rr[:], AF.Exp, R=[rr, n16], W=[rr], scale=n16[:, d, g:g + 1])
                kb.act(rr[:], rr[:], AF.Sqrt, R=[rr], W=[rr], bias=1.0, scale=-1.0)
                kb.tt(ii[:], ii[:], xr[:], ALU.mult, R=[ii, xr], W=[ii])
                kb.tt(ii[:], ii[:], rr[:], ALU.mult, R=[ii, rr], W=[ii], eng="pool")
                dsth = hh if d == 0 else h2
                if d == 0:
                    kb.op("dve", lambda e: e.tensor_tensor_scan(out=hh[:], data0=aa[:], data1=ii[:], initial=0.0,
                                                                op0=ALU.mult, op1=ALU.add), R=[aa, ii], W=[hh])
                else:
                    kb.op("dve", lambda e: e.tensor_tensor_scan(out=h2[:, NCTX - 1::-1], data0=aa[:, NCTX - 1::-1],
                                                                data1=ii[:, NCTX - 1::-1], initial=0.0,
                                                                op0=ALU.mult, op1=ALU.add), R=[aa, ii], W=[h2])
                    kb.op("dve", lambda e: e.tensor_tensor_scan(out=h2[:, N - 1:NCTX - 1:-1], data0=aa[:, N - 1:NCTX - 1:-1],
                                                                data1=ii[:, N - 1:NCTX - 1:-1], initial=h2[:, 0:1],
                                                                op0=ALU.mult, op1=ALU.add), R=[aa, ii, h2], W=[h2])
            kb.tt(hh[:], hh[:], h2[:], ALU.add, R=[hh, h2], W=[hh])
            kb.tt(rr[:], gt[:], gt[:], ALU.mult, R=[gt], W=[rr], eng="pool")
            kb.ts(rr[:], rr[:], 0.044715, 1.0, ALU.mult, ALU.add, R=[rr], W=[rr])
            kb.tt(rr[:], rr[:], gt[:], ALU.mult, R=[rr, gt], W=[rr], eng="pool")
            kb.act(rr[:], rr[:], AF.Sigmoid, R=[rr], W=[rr], scale=1.5957691216057308)
            kb.tt(rr[:], rr[:], gt[:], ALU.mult, R=[rr, gt], W=[rr])
            kb.tt(yb[:], rr[:], hh[:], ALU.mult, R=[rr, hh], W=[yb])
            kb.dma("sp", yT_d[4 + g, :, :], yb[:], R=[yb], W=[kb.tok("yT_d", 4 + g)])


def stage_att(kb, cx, l, heads=(0, 1, 2, 3), with_ctx=True):
    uT_d = kb.dram("uT_d", (36, 128, NTOK), F32)
    v_d = kb.dram("v_d", (NTOK, 1024), BF16)
    yT_d = kb.dram("yT_d", (16, 128, NTOK), BF16)
    lvd = kb.inp("att_lambda", (DEPTH, 4, 128))
    sgd = kb.inp("att_subln", (DEPTH, 256))
    cosd = kb.inp("rope_cos", (128, NLAT))
    sind = kb.inp("rope_sin", (128, NLAT))
    lam_init = 0.8 - 0.6 * math.exp(-0.3 * l)
    with kb.scope():
        ident = load_const(kb, cx, "ident_b")
        ones = load_const(kb, cx, "ones_f")
        P = load_const(kb, cx, "rope_P")
        cos = kb.tile((NLAT,))
        sin = kb.tile((NLAT,))
        kb.dma("sp", cos[:], cosd.ap(), R=[kb.tok("attp")], W=[cos])
        kb.dma("sp", sin[:], sind.ap(), R=[kb.tok("attp")], W=[sin])
        lv = kb.tile((4,))
        kb.dma("sp", lv[:], lvd[l].rearrange("r d -> d r"), R=[kb.tok("attp")], W=[lv], allow_slow_non_contiguous=True)
        pr = kb.tile((2,))
        kb.tt(pr[:, 0:1], lv[:, 0:1], lv[:, 1:2], ALU.mult, R=[lv], W=[pr])
        kb.tt(pr[:, 1:2], lv[:, 2:3], lv[:, 3:4], ALU.mult, R=[lv], W=[pr])
        kb.mm(kb.bank[7][:, 0:2], ones[:], pr[:, 0:2], True, True, R=[ones, pr], W=[kb.bank[7]])
        lam = kb.tile((2,))
        kb.act(lam[:], kb.bank[7][:, 0:2], AF.Exp, R=[kb.bank[7]], W=[lam])
        nlam = kb.tile((1,))
        kb.tt(nlam[:], lam[:, 1:2], lam[:, 0:1], ALU.subtract, R=[lam], W=[nlam])
        kb.ts(nlam[:], nlam[:], -lam_init, None, ALU.add, None, R=[nlam], W=[nlam])
        gain = kb.tile((256,))
        kb.dma("sp", gain[:], sgd[l, :].partition_broadcast(128), R=[kb.tok("attp")], W=[gain])
        kb.ts(gain[:], gain[:], 1.0 - lam_init, None, ALU.mult, None, R=[gain], W=[gain])
        raw = kb.tile((NTOK,))
        rot = kb.tile((NLAT,))
        qT = [kb.tile((NTOK,), BF16) for _ in range(2)]
        kT = [kb.tile((NTOK,), BF16) for _ in range(2)]
        vaug = kb.tile((NTT, 257), BF16)
        et = [kb.tile((256,), BF16) for _ in range(4)]
        r12 = kb.tile((4,))
        osb = kb.tile((256,))
        sq = kb.tile((256,))
        ss = kb.tile((1,))
        ob = kb.tile((256,), BF16)
        yTt = [kb.tile((2, 128), BF16) for _ in range(2)]
        cnt = 0
        nout = 0
        for hd in heads:
            for m in range(2):
                for (grp, dst) in ((20 + 2 * hd + m, qT[m]), (28 + 2 * hd + m, kT[m])):
                    kb.dma("sp", raw[:], uT_d[grp, :, :], R=[kb.tok("uT_d", grp)], W=[raw])
                    for i in range(8):
                        bk = kb.bank[4 + i % 3]
                        kb.mm(bk[:, :], P[:], raw[:, NCTX + i * 512:NCTX + (i + 1) * 512], True, True, R=[P, raw], W=[bk])
                        kb.tt(rot[:, i * 512:(i + 1) * 512], bk[:, :], sin[:, i * 512:(i + 1) * 512], ALU.mult, R=[bk, sin], W=[rot])
                    kb.copy(dst[:, 0:NCTX], raw[:, 0:NCTX], R=[raw], W=[dst], eng="act")
                    kb.tt(raw[:, NCTX:], raw[:, NCTX:], cos[:], ALU.mult, R=[raw, cos], W=[raw], eng="pool")
                    kb.tt(dst[:, NCTX:], raw[:, NCTX:], rot[:], ALU.add, R=[raw, rot], W=[dst])
            kb.dma("sp", vaug[:, :, 0:256], v_d[:, hd * 256:(hd + 1) * 256].rearrange("(c p) e -> p c e", p=128),
                   R=[kb.tok("v_d")], W=[vaug])
            kb.memset(vaug[:, :, 256:257], 1.0, W=[vaug])
            qtiles = ([(0, 2)] if with_ctx else []) + [(NCTX + i * 256, NTT) for i in range(16)]
            for (q0, nkc) in qtiles:
                for kc in range(nkc):
                    for m in range(2):
                        sb = kb.bank[4 + cnt % 3]
                        e_ = et[cnt % 4]
                        cnt += 1
                        kb.mm(sb[:, 0:256], kT[m][:, kc * 128:(kc + 1) * 128], qT[m][:, q0:q0 + 256], True, True,
                              R=[kT[m], qT[m]], W=[sb])
                        kb.act(e_[:], sb[:, 0:256], AF.Exp, R=[sb], W=[e_], scale=128.0 ** -0.5)
                        for qb in range(2):
                            ob_ = kb.bank[m * 2 + qb]
                            kb.mm(ob_[:, 0:257], e_[:, qb * 128:(qb + 1) * 128], vaug[:, kc, :], (kc == 0), (kc == nkc - 1),
                                  R=[e_, vaug], W=[ob_])
                for qb in range(2):
                    O1 = kb.bank[qb]
                    O2 = kb.bank[2 + qb]
                    kb.op("dve", lambda e, O1=O1: e.reciprocal(out=r12[:, 0:1], in_=O1[:, 256:257]), R=[O1], W=[r12])
                    kb.op("dve", lambda e, O2=O2: e.reciprocal(out=r12[:, 1:2], in_=O2[:, 256:257]), R=[O2], W=[r12])
                    kb.tt(r12[:, 2:3], r12[:, 1:2], nlam[:, 0:1], ALU.mult, R=[r12, nlam], W=[r12])
                    kb.ts(osb[:], O1[:, 0:256], r12[:, 0:1], None, ALU.mult, None, R=[O1, r12], W=[osb])
                    kb.stt(osb[:], O2[:, 0:256], r12[:, 2:3], osb[:], ALU.mult, ALU.add, R=[O2, r12, osb], W=[osb])
                    kb.tt(sq[:], osb[:], osb[:], ALU.mult, R=[osb], W=[sq], eng="pool")
                    kb.op("dve", lambda e: e.reduce_sum(out=ss[:, 0:1], in_=sq[:], axis=AX.X), R=[sq], W=[ss])
                    kb.act(ss[:], ss[:], AF.Sqrt, R=[ss, kb.eps], W=[ss], bias=kb.eps[:, 0:1], scale=1.0 / 256.0)
                    kb.op("dve", lambda e: e.reciprocal(out=ss[:], in_=ss[:]), R=[ss], W=[ss])
                    kb.ts(osb[:], osb[:], ss[:, 0:1], None, ALU.mult, None, R=[osb, ss], W=[osb])
                    kb.tt(ob[:], osb[:], gain[:], ALU.mult, R=[osb, gain], W=[ob])
                    bk7 = kb.bank[7]
                    pv = bk7.ap.bitcast(BF16).rearrange("p (a b) -> p a b", b=128)
                    yt_ = yTt[nout % 2]
                    nout += 1
                    for j in range(2):
                        kb.tr(pv[:, j, :], ob[:, j * 128:(j + 1) * 128], ident[:], R=[ob, ident], W=[bk7])
                    kb.copy(yt_[:], pv[:, 0:2, :], R=[bk7], W=[yt_], eng="act")
                    t0 = q0 + qb * 128
                    kb.dma("sp", yT_d[8 + 2 * hd:10 + 2 * hd, :, t0:t0 + 128].rearrange("j p t -> p j t"), yt_[:],
                           R=[yt_], W=[kb.tok("yT_d", 8 + 2 * hd), kb.tok("yT_d", 9 + 2 * hd)])


def stage_b1(kb, cx, l, src_name, tt0):
    x_d = kb.inp(src_name, (NTOK, D)) if src_name == "xin" else kb.dram(src_name, (NTOK, D), F32)
    yT_d = kb.dram("yT_d", (16, 128, NTOK), BF16)
    x1_d = kb.dram("x1_d", (NTOK, D), F32)
    w_out = kb.inp("w_out", (DEPTH, D, D))
    lng = kb.inp("ln_g", (DEPTH, 2, D))
    lnb = kb.inp("ln_b", (DEPTH, 2, D))
    with kb.scope():
        wo = kb.tile((16, 2048), BF16)
        for kc in range(16):
            kb.dma("pool", wo[:, kc, :], w_out[l, kc * 128:(kc + 1) * 128, :], R=[kb.tok("w_out")], W=[wo])
        g1 = [kb.tile((2048,)) for _ in range(2)]
        for r in range(2):
            load_mod(kb, l, 2, r, g1[r])
        gg = kb.tile((2048,))
        bb = kb.tile((2048,))
        kb.dma("sp", gg[:], lng[l, 0, :].partition_broadcast(128), R=[kb.tok("lnp")], W=[gg])
        kb.dma("sp", bb[:], lnb[l, 0, :].partition_broadcast(128), R=[kb.tok("lnp")], W=[bb])
        yt = [kb.tile((16, 128), BF16) for _ in range(2)]
        xt = [kb.tile((2048,)) for _ in range(2)]
        xo = [kb.tile((2048,)) for _ in range(2)]
        t = kb.tile((2048,))
        st = kb.tile((4, 6))
        mv = kb.tile((2,))
        rstd = kb.tile((1,))
        for tt in range(tt0, NTT):
            r = 1 if tt < 2 else 0
            y = yt[tt % 2]
            x = xt[tt % 2]
            kb.dma("sp", y[:], yT_d[:, :, tt * 128:(tt + 1) * 128].rearrange("kc p t -> p kc t"),
                   R=[kb.tok("yT_d", i) for i in range(16)], W=[y])
            kb.dma("sp", x[:], x_d[tt * 128:(tt + 1) * 128, :], R=[kb.tok(src_name, tt)], W=[x])
            for ct in range(4):
                bk = kb.bank[(tt % 2) * 4 + ct]
                for kc in range(16):
                    kb.mm(bk[:, :], y[:, kc, :], wo[:, kc, ct * 512:(ct + 1) * 512], (kc == 0), (kc == 15), R=[y, wo], W=[bk])
                kb.tt(t[:, ct * 512:(ct + 1) * 512], bk[:, :], g1[r][:, ct * 512:(ct + 1) * 512], ALU.mult, R=[bk, g1[r]], W=[t])
            kb.stt(t[:], x[:], ALPHA, t[:], ALU.mult, ALU.add, R=[x, t], W=[t])
            layer_norm_tile(kb, t, 128, st, mv, rstd)
            kb.ts(t[:], t[:], mv[:, 0:1], rstd[:, 0:1], ALU.subtract, ALU.mult, R=[t, mv, rstd], W=[t])
            kb.tt(t[:], t[:], gg[:], ALU.mult, R=[t, gg], W=[t], eng="pool")
            o = xo[tt % 2]
            kb.tt(o[:], t[:], bb[:], ALU.add, R=[t, bb], W=[o])
            kb.dma("sp", x1_d[tt * 128:(tt + 1) * 128, :], o[:], R=[o], W=[kb.tok("x1_d", tt)])


def stage_moe(kb, cx, l, tt0, final, experts=65):
    fT_d = kb.dram("fT_d", (128, 16, NTOK), BF16)
    x1_d = kb.dram("x1_d", (NTOK, D), F32)
    if final:
        kb.io_out.add("out")
        dst_d = kb.dram("out", (NLAT, D), F32)
    else:
        dst_d = kb.dram("xres", (NTOK, D), F32)
    rw = kb.inp("router_w", (DEPTH, D, NEXP))
    rb = kb.inp("router_b", (DEPTH, NEXP))
    wg = kb.inp("exp_w_gate", (DEPTH, NEXP, D, DEXP))
    wu = kb.inp("exp_w_up", (DEPTH, NEXP, D, DEXP))
    wd = kb.inp("exp_w_down", (DEPTH, NEXP, DEXP, D))
    sg = kb.inp("sh_w_gate", (DEPTH, D, DEXP))
    su = kb.inp("sh_w_up", (DEPTH, D, DEXP))
    sd = kb.inp("sh_w_down", (DEPTH, DEXP, D))
    lng = kb.inp("ln_g", (DEPTH, 2, D))
    lnb = kb.inp("ln_b", (DEPTH, 2, D))
    with kb.scope():
        ident = load_const(kb, cx, "ident_b")
        G = kb.tile((NTT, 65))
        with kb.scope():
            rwt = kb.tile((16, 64), BF16)
            kb.dma("pool", rwt[:], rw[l].rearrange("(kc p) e -> p kc e", p=128), R=[kb.tok("rw")], W=[rwt])
            rbt = kb.tile((64,))
            kb.dma("sp", rbt[:], rb[l, :].partition_broadcast(128), R=[kb.tok("rw")], W=[rbt])
            ft = [kb.tile((16, 128), BF16) for _ in range(2)]
            sc = kb.tile((64,))
            sbb = kb.tile((64,))
            top = kb.tile((8,))
            den = kb.tile((2,))
            for tt in range(tt0, NTT):
                f = ft[tt % 2]
                kb.dma("sp", f[:], fT_d[:, :, tt * 128:(tt + 1) * 128], R=[kb.tok("fT_d", tt // 4)], W=[f])
                bk = kb.bank[tt % 2]
                for kc in range(16):
                    kb.mm(bk[:, 0:64], f[:, kc, :], rwt[:, kc, :], (kc == 0), (kc == 15), R=[f, rwt], W=[bk])
                kb.act(sc[:], bk[:, 0:64], AF.Sigmoid, R=[bk], W=[sc])
                kb.tt(sbb[:], sc[:], rbt[:], ALU.add, R=[sc, rbt], W=[sbb])
                kb.op("dve", lambda e: e.max(out=top[:], in_=sbb[:]), R=[sbb], W=[top])
                kb.ts(sbb[:], sbb[:], top[:, 7:8], None, ALU.is_ge, None, R=[sbb, top], W=[sbb])
                kb.tt(sbb[:], sbb[:], sc[:], ALU.mult, R=[sbb, sc], W=[sbb])
                kb.op("dve", lambda e: e.reduce_sum(out=den[:, 0:1], in_=sbb[:], axis=AX.X), R=[sbb], W=[den])
                kb.op("dve", lambda e: e.reciprocal(out=den[:, 1:2], in_=den[:, 0:1]), R=[den], W=[den])
                kb.ts(G[:, tt, 0:64], sbb[:], den[:, 1:2], 2.5, ALU.mult, ALU.mult, R=[sbb, den], W=[G])
                kb.memset(G[:, tt, 64:65], 1.0, W=[G])
        acc = [kb.tile((2048,)) for _ in range(8)]
        for c0 in range(tt0, NTT, 8):
            tiles = list(range(c0, min(c0 + 8, NTT)))
            with kb.scope():
                wgt = kb.tile((16, 512), BF16)
                wut = kb.tile((16, 512), BF16)
                wdt = kb.tile((4, 2048), BF16)
                stg = [kb.tile((4, 512)) for _ in range(2)]
                ft = [kb.tile((16, 128), BF16) for _ in range(2)]
                a_sb = kb.tile((512,))
                ab = [kb.tile((512,), BF16) for _ in range(2)]
                aT = [kb.tile((4, 128), BF16) for _ in range(2)]
                ns = 0
                n = 0
                for e in range(experts):
                    kb.checkpoint()
                    gsrc = wg[l, e] if e < 64 else sg[l]
                    usrc = wu[l, e] if e < 64 else su[l]
                    dsrc = wd[l, e] if e < 64 else sd[l]
                    for (src, dstw) in ((gsrc, wgt), (usrc, wut)):
                        for q4 in range(4):
                            s_ = stg[ns % 2]
                            ns += 1
                            kb.dma("sp", s_[:], src[q4 * 512:(q4 + 1) * 512, :].rearrange("(kc p) c -> p kc c", p=128),
                                   R=[kb.tok("expw")], W=[s_])
                            kb.copy(dstw[:, q4 * 4:(q4 + 1) * 4, :], s_[:], R=[s_], W=[dstw], eng="pool")
                    for k4 in range(4):
                        s_ = stg[ns % 2]
                        ns += 1
                        kb.dma("sp", s_[:].rearrange("p a b -> p (a b)"), dsrc[k4 * 128:(k4 + 1) * 128, :], R=[kb.tok("expw")], W=[s_])
                        kb.copy(wdt[:, k4, :], s_[:].rearrange("p a b -> p (a b)"), R=[s_], W=[wdt], eng="pool")
                    for tt in tiles:
                        f = ft[n % 2]
                        a_b = ab[n % 2]
                        a_t = aT[n % 2]
                        n += 1
                        kb.dma("sp", f[:], fT_d[:, :, tt * 128:(tt + 1) * 128], R=[kb.tok("fT_d", tt // 4)], W=[f])
                        for kc in range(16):
                            kb.mm(kb.bank[0][:, :], f[:, kc, :], wgt[:, kc, :], (kc == 0), (kc == 15), R=[f, wgt], W=[kb.bank[0]])
                        for kc in range(16):
                            kb.mm(kb.bank[1][:, :], f[:, kc, :], wut[:, kc, :], (kc == 0), (kc == 15), R=[f, wut], W=[kb.bank[1]])
                        kb.act(a_sb[:], kb.bank[0][:, :], AF.Silu, R=[kb.bank[0]], W=[a_sb])
                        kb.stt(a_b[:], a_sb[:], G[:, tt, e:e + 1], kb.bank[1][:, :], ALU.mult, ALU.mult, R=[a_sb, G, kb.bank[1]], W=[a_b])
                        b2 = kb.bank[2]
                        pv = b2.ap.bitcast(BF16).rearrange("p (a b) -> p a b", b=128)
                        for k4 in range(4):
                            kb.tr(pv[:, k4, :], a_b[:, k4 * 128:(k4 + 1) * 128], ident[:], R=[a_b, ident], W=[b2])
                        kb.copy(a_t[:], pv[:, 0:4, :], R=[b2], W=[a_t], eng="act")
                        a = acc[tt - c0]
                        for ct in range(4):
                            bk = kb.bank[3 + ct]
                            for k4 in range(4):
                                kb.mm(bk[:, :], a_t[:, k4, :], wdt[:, k4, ct * 512:(ct + 1) * 512], (k4 == 0), (k4 == 3), R=[a_t, wdt], W=[bk])
                            if e == 0:
                                kb.copy(a[:, ct * 512:(ct + 1) * 512], bk[:, :], R=[bk], W=[a])
                            else:
                                kb.tt(a[:, ct * 512:(ct + 1) * 512], a[:, ct * 512:(ct + 1) * 512], bk[:, :], ALU.add, R=[a, bk], W=[a])
            with kb.scope():
                g2 = [kb.tile((2048,)) for _ in range(2)]
                for r in range(2):
                    load_mod(kb, l, 5, r, g2[r])
                gg = kb.tile((2048,))
                bb = kb.tile((2048,))
                kb.dma("sp", gg[:], lng[l, 1, :].partition_broadcast(128), R=[kb.tok("lnp")], W=[gg])
                kb.dma("sp", bb[:], lnb[l, 1, :].partition_broadcast(128), R=[kb.tok("lnp")], W=[bb])
                xt = [kb.tile((2048,)) for _ in range(2)]
                st = kb.tile((4, 6))
                mv = kb.tile((2,))
                rstd = kb.tile((1,))
                for tt in tiles:
                    r = 1 if tt < 2 else 0
                    x = xt[tt % 2]
                    a = acc[tt - c0]
                    kb.dma("sp", x[:], x1_d[tt * 128:(tt + 1) * 128, :], R=[kb.tok("x1_d", tt)], W=[x])
                    kb.tt(a[:], a[:], g2[r][:], ALU.mult, R=[a, g2[r]], W=[a])
                    kb.stt(a[:], x[:], ALPHA, a[:], ALU.mult, ALU.add, R=[x, a], W=[a])
                    layer_norm_tile(kb, a, 128, st, mv, rstd)
                    kb.ts(a[:], a[:], mv[:, 0:1], rstd[:, 0:1], ALU.subtract, ALU.mult, R=[a, mv, rstd], W=[a])
                    kb.tt(a[:], a[:], gg[:], ALU.mult, R=[a, gg], W=[a], eng="pool")
                    kb.tt(a[:], a[:], bb[:], ALU.add, R=[a, bb], W=[a])
                    if final:
                        kb.dma("sp", dst_d[(tt - 2) * 128:(tt - 1) * 128, :], a[:], R=[a], W=[kb.tok("out", tt)])
                    else:
                        kb.dma("sp", dst_d[tt * 128:(tt + 1) * 128, :], a[:], R=[a], W=[kb.tok("xres", tt)])


def full_stages():
    st = [lambda kb, cx: stage_mods(kb, cx)]
    for l in range(DEPTH):
        src = "xin" if l == 0 else "xres"
        wc = (l == 0)
        tt0 = 0 if l == 0 else 2
        st.append(lambda kb, cx, l=l, src=src: stage_a1(kb, cx, l, src))
        st.append(lambda kb, cx, l=l: stage_a1b(kb, cx, l))
        st.append(lambda kb, cx, l=l, wc=wc: stage_hyena(kb, cx, l, with_ctx=wc))
        st.append(lambda kb, cx, l=l: stage_lru(kb, cx, l))
        st.append(lambda kb, cx, l=l, wc=wc: stage_att(kb, cx, l, with_ctx=wc))
        st.append(lambda kb, cx, l=l, src=src, tt0=tt0: stage_b1(kb, cx, l, src, tt0))
        st.append(lambda kb, cx, l=l, tt0=tt0: stage_a1(kb, cx, l, "x1_d", jsh=3, jsc=4, dst="fT_d", tt0=tt0))
        st.append(lambda kb, cx, l=l, tt0=tt0: stage_moe(kb, cx, l, tt0, final=(l == DEPTH - 1)))
    return st


def build(stages, io_in=(), io_out=()):
    kb = KB(io_in, io_out)
    cx = Ctx()
    for s in stages:
        s(kb, cx)
    nc = kb.emit()
    return kb, nc


def kernel(**inputs):
    kb, nc = build(full_stages())
    hc = host_consts()
    B = inputs["x"].shape[0]
    in_maps = []
    for b in range(B):
        full = {
            "xin": np.ascontiguousarray(np.concatenate([inputs["ctx"][b], inputs["x"][b]], axis=0), dtype=np.float32),
            "crows": np.ascontiguousarray(np.stack([inputs["c"][b], inputs["c_ctx"]], axis=0), dtype=np.float32),
        }
        im = {}
        for name, (shape, dt) in kb.inputs.items():
            if name in full:
                a = full[name]
            elif name in hc:
                a = hc[name]
            else:
                a = np.ascontiguousarray(inputs[name])
            assert tuple(a.shape) == tuple(shape), (name, a.shape, shape)
            im[name] = a
        in_maps.append(im)
    res = run_bass_kernel_spmd(nc, in_maps, core_ids=list(range(B)))
    out = np.stack([np.asarray(res.results[b]["out"], dtype=np.float32) for b in range(B)], axis=0)
    return out
```
